# Optimizing a Trainium2 kernel written in Bass

```python
import jax, jax.numpy as jnp
from jax import lax
import numpy as np

D_MODEL = 2048
BATCH = 8
SEQ = 2048
DEPTH = 2

GLA_HEADS = 4
GLA_KEY_W = D_MODEL // 4
GLA_VAL_W = D_MODEL // 2
GLA_DK = GLA_KEY_W // GLA_HEADS
GLA_DV = GLA_VAL_W // GLA_HEADS
GLA_GATE_RANK = 16
GLA_GATE_NORM = 16.0
GLA_CHUNK = 64
MOBA_HEAD_DIM = 128
MOBA_W = D_MODEL // 2
MOBA_HEADS = MOBA_W // MOBA_HEAD_DIM
MOBA_BLOCK = 256
MOBA_TOPK = 3
MOBA_QCHUNK = 16
D_FF = ((8 * D_MODEL // 3 + 255) // 256) * 256
N_BRANCH = 2
EPS = 1e-6
IN_SPLITS = (GLA_KEY_W, GLA_KEY_W, GLA_VAL_W, GLA_VAL_W, GLA_GATE_RANK, MOBA_W, MOBA_W, MOBA_W, N_BRANCH * D_MODEL)
IN_COLS = GLA_KEY_W * 2 + GLA_VAL_W * 2 + GLA_GATE_RANK + MOBA_W * 3 + N_BRANCH * D_MODEL

kernel_name = 'hybrid_gla_moba_adaln_block'


def rms_norm(x, g):
    x32 = x.astype(jnp.float32)
    y = x32 * lax.rsqrt(jnp.mean(x32 * x32, axis=-1, keepdims=True) + EPS)
    return (y * g.astype(jnp.float32)).astype(x.dtype)


def gla_chunk_step(state, inp):
    q, k, v, g = inp
    C = q.shape[2]
    b = jnp.cumsum(g, axis=2)
    causal = jnp.tril(jnp.ones((C, C), dtype=bool))
    diff = b[:, :, :, None, :] - b[:, :, None, :, :]
    decay = jnp.exp(jnp.where(causal[None, None, :, :, None], diff, -jnp.inf))
    attn = jnp.einsum('bhtsd,bhsd->bhts', q[:, :, :, None, :] * decay, k)
    o = (jnp.einsum('bhts,bhsv->bhtv', attn, v)
         + jnp.einsum('bhtd,bhdv->bhtv', q * jnp.exp(b), state))
    b_last = b[:, :, -1:, :]
    state = (jnp.exp(b_last[:, :, 0, :])[..., None] * state
             + jnp.einsum('bhsd,bhsv->bhdv', k * jnp.exp(b_last - b), v))
    return state, o


def gla_mixer(q, k, v, log_a):
    B, S = q.shape[0], q.shape[1]
    nc = S // GLA_CHUNK

    def to_chunks(t):
        t = t.astype(jnp.float32).reshape(B, nc, GLA_CHUNK, GLA_HEADS, t.shape[-1])
        return jnp.transpose(t, (1, 0, 3, 2, 4))

    q = q * (GLA_DK ** -0.5)
    state0 = jnp.zeros((B, GLA_HEADS, GLA_DK, GLA_DV), jnp.float32)
    _, o = lax.scan(gla_chunk_step, state0,
                    (to_chunks(q), to_chunks(k), to_chunks(v), to_chunks(log_a)))
    return jnp.transpose(o, (1, 0, 3, 2, 4)).reshape(B, S, GLA_HEADS, GLA_DV)


def moba_mixer(q, k, v):
    B, S = q.shape[0], q.shape[1]
    H, hd, BS, QC = MOBA_HEADS, MOBA_HEAD_DIM, MOBA_BLOCK, MOBA_QCHUNK
    nb = -(-S // BS)
    sp = nb * BS
    pad = ((0, 0), (0, sp - S), (0, 0), (0, 0))
    q = jnp.transpose(jnp.pad(q, pad), (0, 2, 1, 3)) * (hd ** -0.5)
    k = jnp.transpose(jnp.pad(k, pad), (0, 2, 1, 3))
    v = jnp.transpose(jnp.pad(v, pad), (0, 2, 1, 3))
    kb = k.reshape(B, H, nb, BS, hd)
    vb = v.reshape(B, H, nb, BS, hd)
    k_mean = jnp.mean(kb.astype(jnp.float32), axis=3)
    q_block = jnp.arange(sp) // BS
    past = jnp.arange(nb)[None, :] < q_block[:, None]
    blk_score = jnp.einsum('bhsd,bhnd->bhsn', q.astype(jnp.float32), k_mean)
    blk_score = jnp.where(past[None, None], blk_score, -jnp.inf)
    topk = min(MOBA_TOPK, nb)
    _, sel = lax.top_k(blk_score, topk)
    nq = sp // QC

    def chunks(t):
        return jnp.moveaxis(t.reshape(B, H, nq, QC, t.shape[-1]), 2, 0)

    bi = jnp.arange(B)[:, None, None, None]
    hi = jnp.arange(H)[None, :, None, None]

    def attend(args):
        qc, selc, ci = args
        own = (ci * QC) // BS
        k_sel = kb[bi, hi, selc]
        v_sel = vb[bi, hi, selc]
        k_own = lax.dynamic_index_in_dim(kb, own, axis=2, keepdims=False)
        v_own = lax.dynamic_index_in_dim(vb, own, axis=2, keepdims=False)
        s_sel = jnp.einsum('bhqd,bhqjtd->bhqjt', qc, k_sel).reshape(B, H, QC, topk * BS)
        s_own = jnp.einsum('bhqd,bhtd->bhqt', qc, k_own)
        sel_ok = jnp.repeat(selc < own, BS, axis=-1)
        q_pos = ci * QC + jnp.arange(QC)
        k_pos = own * BS + jnp.arange(BS)
        own_ok = k_pos[None, :] <= q_pos[:, None]
        scores = jnp.concatenate(
            [jnp.where(sel_ok, s_sel.astype(jnp.float32), -jnp.inf),
             jnp.where(own_ok[None, None], s_own.astype(jnp.float32), -jnp.inf)], axis=-1)
        p = jax.nn.softmax(scores, axis=-1).astype(v.dtype)
        p_sel = p[..., :topk * BS].reshape(B, H, QC, topk, BS)
        return (jnp.einsum('bhqjt,bhqjtd->bhqd', p_sel, v_sel)
                + jnp.einsum('bhqt,bhtd->bhqd', p[..., topk * BS:], v_own))

    o = lax.map(attend, (chunks(q), chunks(sel), jnp.arange(nq)))
    o = jnp.moveaxis(o, 0, 2).reshape(B, H, sp, hd)[:, :, :S]
    return jnp.transpose(o, (0, 2, 1, 3))


def setup_inputs(seed: int = 0) -> dict:
    key = jax.random.key(seed)
    ks = jax.random.split(key, 16)

    def w(k, shape, fan_in):
        return jax.random.normal(k, shape, jnp.float32) * (fan_in ** -0.5)

    def gain(k, shape):
        return 1.0 + 0.05 * jax.random.normal(k, shape, jnp.float32)

    return {
        'x': jax.random.normal(ks[0], (BATCH, SEQ, D_MODEL), jnp.float32),
        'c': jax.random.normal(ks[1], (BATCH, D_MODEL), jnp.float32),
        'ada_w': w(ks[2], (DEPTH, D_MODEL, 6 * D_MODEL), D_MODEL),
        'ada_b': 0.02 * jax.random.normal(ks[3], (DEPTH, 6 * D_MODEL), jnp.float32),
        'norm1_g': gain(ks[4], (DEPTH, D_MODEL)),
        'w_in': w(ks[5], (DEPTH, D_MODEL, IN_COLS), D_MODEL),
        'gla_gate_w2': w(ks[6], (DEPTH, GLA_GATE_RANK, GLA_KEY_W), GLA_GATE_RANK),
        'gla_gate_b': 0.02 * jax.random.normal(ks[7], (DEPTH, GLA_KEY_W), jnp.float32),
        'gla_norm_g': gain(ks[8], (DEPTH, GLA_DV)),
        'w_up_gla': w(ks[9], (DEPTH, GLA_VAL_W, D_MODEL), GLA_VAL_W),
        'w_up_moba': w(ks[10], (DEPTH, MOBA_W, D_MODEL), MOBA_W),
        'w_out': w(ks[11], (DEPTH, D_MODEL, D_MODEL), D_MODEL),
        'norm2_g': gain(ks[12], (DEPTH, D_MODEL)),
        'w_ffn_in': w(ks[13], (DEPTH, D_MODEL, 2 * D_FF), D_MODEL),
        'w_ffn_out': w(ks[14], (DEPTH, D_FF, D_MODEL), D_FF),
        'final_g': gain(ks[15], (D_MODEL,)),
    }


def reference(x, c, ada_w, ada_b, norm1_g, w_in, gla_gate_w2, gla_gate_b, gla_norm_g,
              w_up_gla, w_up_moba, w_out, norm2_g, w_ffn_in, w_ffn_out, final_g):
    B, S = x.shape[0], x.shape[1]
    split_pts = np.cumsum(np.array(IN_SPLITS))[:-1].tolist()
    c_act = jax.nn.silu(c)
    for l in range(DEPTH):
        mod = c_act @ ada_w[l] + ada_b[l]
        sh1, sc1, gt1, sh2, sc2, gt2 = jnp.split(mod, 6, axis=-1)
        h = rms_norm(x, norm1_g[l]) * (1.0 + sc1[:, None]) + sh1[:, None]
        proj = h @ w_in[l]
        gq, gk, gv, gr, glr, mq, mk, mv, gates = jnp.split(proj, split_pts, axis=-1)
        log_a = jax.nn.log_sigmoid((glr @ gla_gate_w2[l] + gla_gate_b[l]).astype(jnp.float32)) / GLA_GATE_NORM
        o_gla = gla_mixer(gq.reshape(B, S, GLA_HEADS, GLA_DK), gk.reshape(B, S, GLA_HEADS, GLA_DK),
                          gv.reshape(B, S, GLA_HEADS, GLA_DV), log_a.reshape(B, S, GLA_HEADS, GLA_DK))
        o_gla = rms_norm(o_gla.astype(x.dtype), gla_norm_g[l]).reshape(B, S, GLA_VAL_W) * jax.nn.silu(gr)
        y_gla = o_gla @ w_up_gla[l]
        o_moba = moba_mixer(mq.reshape(B, S, MOBA_HEADS, MOBA_HEAD_DIM), mk.reshape(B, S, MOBA_HEADS, MOBA_HEAD_DIM),
                            mv.reshape(B, S, MOBA_HEADS, MOBA_HEAD_DIM)).reshape(B, S, MOBA_W)
        y_moba = o_moba @ w_up_moba[l]
        g_gla, g_moba = jnp.split(jax.nn.sigmoid(gates), 2, axis=-1)
        x = x + gt1[:, None] * ((g_gla * y_gla + g_moba * y_moba) @ w_out[l])
        h = rms_norm(x, norm2_g[l]) * (1.0 + sc2[:, None]) + sh2[:, None]
        a, u = jnp.split(h @ w_ffn_in[l], 2, axis=-1)
        x = x + gt2[:, None] * ((jax.nn.silu(a) * u) @ w_ffn_out[l])
    return rms_norm(x, final_g)
```

```python
import contextlib
import numpy as np
import concourse.bass as bass
import concourse.mybir as mybir
from concourse.alu_op_type import AluOpType as ALU
from concourse.bass_utils import run_bass_kernel_spmd

F32 = mybir.dt.float32
BF16 = mybir.dt.bfloat16
AF = mybir.ActivationFunctionType
AX = mybir.AxisListType

D = 2048
S = 2048
KC = 16
NTC = 4
NTT = 16
DFF = 5632
NJ = 44
DEPTH = 2
EPS = 1e-6
OFF_GQ, OFF_GK, OFF_GV, OFF_GR, OFF_GLR = 0, 512, 1024, 2048, 3072
OFF_MQ, OFF_MK, OFF_MV, OFF_GG, OFF_GM = 3088, 4112, 5136, 6160, 8208
NQ = 3
FUSED_STATS = True

BASE = 16512
R_CONST, R_A, R_OG, R_OM, R_W, R_T = 0, 10240, 75776, 108544, 141312, 165888
NSLOT = 6


def nm(regions, *key):
    if isinstance(regions, str):
        regions = (regions,)
    return (tuple(regions),) + tuple(key)


class Prog:
    ENG = ("pe", "act", "dve", "pool", "sp")

    def __init__(self, nc):
        self.nc = nc
        self.items = {e: [] for e in self.ENG}
        self.cnt = {e: 0 for e in self.ENG}
        self.sems = {}
        self.lastw = {}
        self.readers = {}
        self.waited = {e: {} for e in self.ENG}
        self.guard = {}

    def fence(self, region):
        g = self.guard.setdefault(region, {})
        names = [n for n in set(self.lastw) | set(self.readers) if region in n[0]]
        for n in names:
            toks = []
            t = self.lastw.pop(n, None)
            if t is not None:
                toks.append(t)
            toks.extend(self.readers.pop(n, ()))
            for s, v in toks:
                if g.get(s, 0) < v:
                    g[s] = v

    def op(self, eng, fn, reads=(), writes=(), dma=None):
        need = {}

        def add(t):
            if t is not None and need.get(t[0], 0) < t[1]:
                need[t[0]] = t[1]

        for r in reads:
            add(self.lastw.get(r))
        for w in writes:
            if w not in self.lastw:
                for reg in w[0]:
                    for s, v in self.guard.get(reg, {}).items():
                        add((s, v))
            add(self.lastw.get(w))
            for t in self.readers.get(w, ()):
                add(t)
        wl = []
        for s, v in need.items():
            if self.waited[eng].get(s, 0) < v:
                self.waited[eng][s] = v
                wl.append((s, v))
        if dma is None:
            self.cnt[eng] += 1
            tok = ("E_" + eng, self.cnt[eng])
            inc = (tok[0], 1)
        else:
            self.sems[dma] = self.sems.get(dma, 0) + 16
            tok = (dma, self.sems[dma])
            inc = (dma, 16)
        self.items[eng].append((wl, fn, inc))
        for r in reads:
            self.readers.setdefault(r, []).append(tok)
        for w in writes:
            self.lastw[w] = tok
            self.readers[w] = []
        return tok

    def emit(self, final_tokens):
        nc = self.nc
        names = set("E_" + e for e in self.ENG)
        names.update(self.sems.keys())
        names = sorted(names)
        with contextlib.ExitStack() as st:
            semo = {n: st.enter_context(nc.semaphore(n)) for n in names}
            block = st.enter_context(nc.Block())
            engmap = {"pe": "tensor", "act": "scalar", "dve": "vector", "pool": "gpsimd", "sp": "sync"}

            def mk(e):
                def body(eng):
                    for (wl, fn, inc) in self.items[e]:
                        for s, v in wl:
                            eng.wait_ge(semo[s], v)
                        inst = fn(eng)
                        inst.then_inc(semo[inc[0]], inc[1])
                    if e == "sp":
                        for (s, v) in final_tokens:
                            eng.wait_ge(semo[s], v)
                return body

            for e in self.ENG:
                if self.items[e] or e == "sp":
                    getattr(block, engmap[e])(mk(e))


class Job:
    def __init__(self, loads, fn):
        self.loads = loads
        self.fn = fn


def build_program(stop=None, dbg_names=()):
    nc = bass.Bass("TRN2", target_bir_lowering=False)
    P = Prog(nc)

    def din(name, shape):
        return nc.dram_tensor(name, list(shape), F32, kind="ExternalInput").ap()

    x_d = din("x", [S, D])
    c_d = din("c", [D])
    ada_w = din("ada_w", [DEPTH, D, 6 * D])
    ada_b = din("ada_b", [DEPTH, 6 * D])
    norm1_g = din("norm1_g", [DEPTH, D])
    w_in = din("w_in", [DEPTH, D, 10256])
    gate_w2 = din("gla_gate_w2", [DEPTH, 16, 512])
    gate_b = din("gla_gate_b", [DEPTH, 512])
    gnorm_g = din("gla_norm_g", [DEPTH, 256])
    w_up_gla = din("w_up_gla", [DEPTH, 1024, D])
    w_up_moba = din("w_up_moba", [DEPTH, 1024, D])
    w_out = din("w_out", [DEPTH, D, D])
    norm2_g = din("norm2_g", [DEPTH, D])
    w_ffn_in = din("w_ffn_in", [DEPTH, D, 2 * DFF])
    w_ffn_out = din("w_ffn_out", [DEPTH, DFF, D])
    final_g = din("final_g", [D])
    y_d = nc.dram_tensor("y", [S, D], F32, kind="ExternalOutput").ap()
    R_d = nc.dram_tensor("resid", [KC, 128, S], F32, kind="Internal").ap()
    MT_d = nc.dram_tensor("mt_scr", [KC, 128, S], BF16, kind="Internal").ap()
    dbg_out = {}

    _cnt = [0]

    def sb(off, shape, dt, name=None):
        _cnt[0] += 1
        return nc.alloc_sbuf_tensor_at("%s_%d" % (name or "t", _cnt[0]), list(shape), dt, offset=BASE + off)

    PS = [nc.alloc_psum_tensor("B%d" % i, [128, 512], F32) for i in range(8)]

    def BK(b):
        return nm("PS", b)

    ident_bf = sb(R_CONST + 0, [128, 128], BF16)
    ident32 = sb(R_CONST + 256, [128, 128], F32)
    ones_bf = sb(R_CONST + 768, [128, 128], BF16)
    ones32 = sb(R_CONST + 1024, [128, 128], F32)
    maskU32 = sb(R_CONST + 1536, [128, 128], F32)
    maskU_bf = sb(R_CONST + 2048, [128, 128], BF16)
    padmask = sb(R_CONST + 2304, [128, 16, 8], F32)
    epsb = sb(R_CONST + 2816, [128, 1], F32)
    cact = sb(R_CONST + 2848, [128, 16], BF16)
    c32 = sb(R_CONST + 2880, [128, 16], F32)
    modv = sb(R_CONST + 2944, [128, 2, 96], F32)
    adab = sb(R_CONST + 3712, [128, 2, 96], F32)
    g1 = sb(R_CONST + 4480, [128, 2, 16], F32)
    g2 = sb(R_CONST + 4608, [128, 2, 16], F32)
    gmod1 = sb(R_CONST + 4736, [128, 2, 16], F32)
    gmod2 = sb(R_CONST + 4864, [128, 2, 16], F32)
    fg = sb(R_CONST + 4992, [128, 16], F32)
    ngateb = sb(R_CONST + 5056, [128, 2, 4], F32)
    gw2_bf = sb(R_CONST + 5120, [16, 2, 512], BF16)
    gnormb = sb(R_CONST + 7168, [128, 2, 256], F32)
    CN = lambda k: nm("C", k)

    hT = sb(R_A, [128, KC, S], BF16, "hT")
    OG = sb(R_OG, [128, 8, S], BF16, "OG")
    OM = sb(R_OM, [128, 8, S], BF16, "OM")
    FF = sb(R_OG, [128, 15, S], BF16, "FF")
    WS = [sb(R_W + 4096 * s, [128, 2048], BF16, "W") for s in range(NSLOT)]

    def dump(name, ap, shape, dt, reads):
        if name not in dbg_names:
            return
        d = nc.dram_tensor("dbg_" + name, list(shape), dt, kind="ExternalOutput").ap()
        dbg_out[name] = d
        t = P.op("sp", lambda e, d=d, ap=ap: e.dma_start(out=d, in_=ap), reads=reads, writes=[nm("DR", "dbg", name)],
                 dma="S_dbg")
        final.append(t)

    final = []

    _sm = [0]

    def small_load(dst, src, wname):
        _sm[0] += 1
        P.op("sp", lambda e: e.dma_start(out=dst, in_=src, allow_slow_non_contiguous=True), writes=[wname],
             dma="S_sm%d" % _sm[0])

    def setup():
        gw2_32 = sb(R_T + 0, [16, 2, 512], F32)
        P.op("pool", lambda e: e.memset(ident32[:], 1.0), writes=[CN("ident32")])
        P.op("pool", lambda e: e.affine_select(out=ident32[:], in_=ident32[:], pattern=[[-1, 128]],
                                               compare_op=ALU.is_equal, fill=0.0, base=0, channel_multiplier=1),
             reads=[CN("ident32")], writes=[CN("ident32")])
        P.op("pool", lambda e: e.memset(maskU32[:], 1.0), writes=[CN("maskU32")])
        P.op("pool", lambda e: e.affine_select(out=maskU32[:], in_=maskU32[:], pattern=[[1, 128]],
                                               compare_op=ALU.is_ge, fill=0.0, base=0, channel_multiplier=-1),
             reads=[CN("maskU32")], writes=[CN("maskU32")])
        P.op("dve", lambda e: e.tensor_copy(out=ident_bf[:], in_=ident32[:]), reads=[CN("ident32")], writes=[CN("ident_bf")])
        P.op("dve", lambda e: e.tensor_copy(out=maskU_bf[:], in_=maskU32[:]), reads=[CN("maskU32")], writes=[CN("maskU_bf")])
        P.op("dve", lambda e: e.memset(ones32[:], 1.0), writes=[CN("ones32")])
        P.op("dve", lambda e: e.memset(ones_bf[:], 1.0), writes=[CN("ones_bf")])
        P.op("dve", lambda e: e.memset(epsb[:], EPS), writes=[CN("epsb")])

        P.op("dve", lambda e: e.memset(padmask[:], -1e30), writes=[CN("padmask")])
        for i in range(1, 8):
            P.op("dve", lambda e, i=i: e.memset(padmask[:, 2 * i:2 * i + 2, 0:i], 0.0), writes=[CN("padmask")])
        small_load(c32[:], c_d.rearrange("(kc p) -> p kc", p=128), CN("c32"))
        P.op("act", lambda e: e.activation(out=cact[:], in_=c32[:], func=AF.Silu), reads=[CN("c32")], writes=[CN("cact")])
        for l in range(DEPTH):
            small_load(adab[:, l, :], ada_b[l].rearrange("(j p) -> p j", p=128), CN("adab%d" % l))
            small_load(g1[:, l, :], norm1_g[l].rearrange("(j p) -> p j", p=128), CN("g1%d" % l))
            small_load(g2[:, l, :], norm2_g[l].rearrange("(j p) -> p j", p=128), CN("g2%d" % l))
            small_load(ngateb[:, l, :], gate_b[l].rearrange("(j p) -> p j", p=128), CN("ngateb%d" % l))
            small_load(gnormb[:, l, :], gnorm_g[l].partition_broadcast(128), CN("gnormb%d" % l))
            P.op("dve", lambda e, l=l: e.tensor_scalar(out=ngateb[:, l, :], in0=ngateb[:, l, :], scalar1=-1.0,
                                                       scalar2=None, op0=ALU.mult),
                 reads=[CN("ngateb%d" % l)], writes=[CN("ngateb%d" % l)])
        small_load(fg[:], final_g.rearrange("(j p) -> p j", p=128), CN("fg"))
        P.op("sp", lambda e: e.dma_start(out=gw2_32[:], in_=gate_w2.rearrange("l k c -> k l c")),
             writes=[nm("T", "gw2_32")], dma="S_smgw")
        P.op("dve", lambda e: e.tensor_copy(out=gw2_bf[:], in_=gw2_32[:]), reads=[nm("T", "gw2_32")], writes=[CN("gw2_bf")])

    jobs = []

    def wblock(w_ap, l, r0, nk, c0, ncols):
        return (w_ap[l, r0:r0 + nk * 128, c0:c0 + ncols].rearrange("(kc p) c -> p kc c", p=128), nk, ncols)

    bank_rot = [0]

    def next_bank(n=4, base=0):
        b = base + bank_rot[0] % n
        bank_rot[0] += 1
        return b

    evac_rot = [0]

    def evac_eng():
        evac_rot[0] += 1
        return "dve" if evac_rot[0] % 2 == 0 else "act"

    def copy_op(eng, out, in_, reads, writes, scale=None):
        if eng == "dve":
            if scale is None:
                P.op("dve", lambda e: e.tensor_copy(out=out, in_=in_), reads=reads, writes=writes)
            else:
                P.op("dve", lambda e: e.tensor_scalar(out=out, in0=in_, scalar1=scale, scalar2=None, op0=ALU.mult),
                     reads=reads, writes=writes)
        else:
            if scale is None:
                P.op("act", lambda e: e.copy(out=out, in_=in_), reads=reads, writes=writes)
            else:
                P.op("act", lambda e: e.activation(out=out, in_=in_, func=AF.Copy, scale=scale), reads=reads, writes=writes)

    HN = [nm("A", "h", cg) for cg in range(KC)]

    def mm_group(bank_ap, M, wv, nk, rhs_fn, reads, bank):
        def mm(e):
            r = None
            for kc in range(nk):
                r = e.matmul(bank_ap, lhsT=wv[:, kc, 0:M], rhs=rhs_fn(kc), start=(kc == 0), stop=(kc == nk - 1))
            return r
        P.op("pe", mm, reads=reads, writes=[BK(bank)])

    def ada_job(l, j):
        def fn(views, wn):
            wv = views[0]
            b = next_bank()
            mm_group(PS[b][:, 0:1], 128, wv, KC, lambda kc: cact[:, kc:kc + 1], [wn[0], CN("cact")], b)
            P.op("dve", lambda e: e.tensor_tensor(out=modv[:, l, j:j + 1], in0=PS[b][:, 0:1], in1=adab[:, l, j:j + 1], op=ALU.add),
                 reads=[BK(b), CN("adab%d" % l)], writes=[CN("mod%d_%d" % (l, j // 16 * 16))])
        return Job([wblock(ada_w, l, 0, KC, j * 128, 128)], fn)

    def gmod_job(l, which):
        def fn(views, wn):
            g, gm, o = (g1, gmod1, 16) if which == 1 else (g2, gmod2, 64)
            P.op("dve", lambda e: e.scalar_tensor_tensor(out=gm[:, l, :], in0=modv[:, l, o:o + 16], scalar=1.0,
                                                         in1=g[:, l, :], op0=ALU.add, op1=ALU.mult),
                 reads=[CN("mod%d_%d" % (l, o)), CN("g%d%d" % (which, l))], writes=[CN("gmod%d%d" % (which, l))])
        return Job([], fn)

    def x0_jobs():
        st = {}

        def mk(cg):
            def fn(views, wn):
                if cg == 0:
                    P.fence("T")
                    st["XS"] = [sb(R_T + 8192 * i, [128, 16, 128], F32) for i in range(2)]
                    st["SL"] = [sb(R_T + 16384 + 8192 * i, [128, S], F32) for i in range(2)]
                    st["SQ"] = [sb(R_T + 32768 + 4096 * i, [128, S], BF16) for i in range(2)]
                xs, sl = st["XS"][cg % 2], st["SL"][cg % 2]
                xn = nm("T", "xs", cg % 2)
                P.op("sp", lambda e: e.dma_start(
                    out=xs[:], in_=x_d[:, cg * 128:(cg + 1) * 128].rearrange("(tt p) f -> p tt f", p=128)),
                    writes=[xn], dma="S_xs%d" % (cg % 2))
                for q in range(4):
                    def tr(e, q=q):
                        r = None
                        for k in range(4):
                            r = e.transpose(out=PS[q][:, k * 128:(k + 1) * 128], in_=xs[:, q * 4 + k, :], identity=ident32[:])
                        return r
                    P.op("pe", tr, reads=[xn, CN("ident32")], writes=[BK(q)])
                    copy_op(evac_eng(), sl[:, q * 512:(q + 1) * 512], PS[q][:], [BK(q)], [nm("T", "sl", cg % 2, q)])
                P.op("sp", lambda e: e.dma_start(out=R_d[cg], in_=sl[:]),
                     reads=[nm("T", "sl", cg % 2, q) for q in range(4)],
                     writes=[nm("DR", "R", cg, tc) for tc in range(NTC)], dma="S_sl%d" % (cg % 2))
                sq = st["SQ"][cg % 2]
                for q in range(4):
                    P.op("act", lambda e, q=q: e.activation(out=sq[:, q * 512:(q + 1) * 512], in_=sl[:, q * 512:(q + 1) * 512], func=AF.Square),
                         reads=[nm("T", "sl", cg % 2, q)], writes=[nm("T", "xsq", cg % 2, q)])
                    P.op("pe", lambda e, q=q: e.matmul(PS[4 + q][:], lhsT=ones_bf[:], rhs=sq[:, q * 512:(q + 1) * 512],
                                                       start=(cg == 0), stop=(cg == KC - 1)),
                         reads=[nm("T", "xsq", cg % 2, q), CN("ones_bf")], writes=[BK(4 + q)])
            return Job([], fn)
        return [mk(cg) for cg in range(KC)]

    def norm_jobs(l, which):
        def fn(views, wn):
            P.fence("T")
            NSL = 4
            SL = [sb(R_T + 8192 * i, [128, S], F32) for i in range(NSL)]
            SQ = [sb(R_T + 16384 + 4096 * i, [128, S], BF16) for i in range(2)]
            RS = sb(R_T + 32768, [128, S], F32)
            if which == 3:
                P.fence("OG")
                YS = [sb(R_OG + 8192 * i, [128, 16, 128], F32) for i in range(2)]
            k = [0]

            def load_slab(cg):
                i = k[0] % NSL
                k[0] += 1
                P.op("sp", lambda e: e.dma_start(out=SL[i][:], in_=R_d[cg]),
                     reads=[nm("DR", "R", cg, tc) for tc in range(NTC)], writes=[nm("T", "nsl", i)], dma="S_nsl%d" % i)
                return i
            for cg in range(KC if not FUSED_STATS else 0):
                i = load_slab(cg)
                P.op("act", lambda e, i=i: e.activation(out=SQ[i][:], in_=SL[i][:], func=AF.Square),
                     reads=[nm("T", "nsl", i)], writes=[nm("T", "nsq", i)])
                for tc in range(NTC):
                    P.op("pe", lambda e, i=i, tc=tc, cg=cg: e.matmul(
                        PS[4 + tc][:], lhsT=ones_bf[:], rhs=SQ[i][:, tc * 512:(tc + 1) * 512],
                        start=(cg == 0), stop=(cg == KC - 1)),
                        reads=[nm("T", "nsq", i), CN("ones_bf")], writes=[BK(4 + tc)])
            for tc in range(NTC):
                P.op("act", lambda e, tc=tc: e.activation(out=RS[:, tc * 512:(tc + 1) * 512], in_=PS[4 + tc][:],
                                                          func=AF.Sqrt, scale=1.0 / D, bias=epsb[:, 0:1]),
                     reads=[BK(4 + tc), CN("epsb")], writes=[nm("T", "rs", tc)])
                P.op("dve", lambda e, tc=tc: e.reciprocal(out=RS[:, tc * 512:(tc + 1) * 512], in_=RS[:, tc * 512:(tc + 1) * 512]),
                     reads=[nm("T", "rs", tc)], writes=[nm("T", "rs", tc)])
            rsn = [nm("T", "rs", tc) for tc in range(NTC)]
            if which == 3:
                scale_ap = lambda cg: fg[:, cg:cg + 1]
                sc_names = [CN("fg")]
            else:
                gm = gmod1 if which == 1 else gmod2
                so = 0 if which == 1 else 48
                scale_ap = lambda cg: gm[:, l, cg:cg + 1]
                sc_names = [CN("gmod%d%d" % (which, l)), CN("mod%d_%d" % (l, so))]
                P.fence("A")
            for cg in range(KC):
                i = load_slab(cg)
                P.op("dve", lambda e, i=i, cg=cg: e.scalar_tensor_tensor(
                    out=SL[i][:], in0=SL[i][:], scalar=scale_ap(cg), in1=RS[:], op0=ALU.mult, op1=ALU.mult),
                    reads=[nm("T", "nsl", i)] + rsn + sc_names, writes=[nm("T", "nsl", i)])
                if which != 3:
                    P.op("act", lambda e, i=i, cg=cg: e.activation(out=hT[:, cg, :], in_=SL[i][:], func=AF.Identity,
                                                                   bias=modv[:, l, so + cg:so + cg + 1]),
                         reads=[nm("T", "nsl", i)] + sc_names, writes=[HN[cg]])
                else:
                    ys = YS[cg % 2]
                    for q in range(4):
                        def tr(e, i=i, q=q):
                            r = None
                            for kk in range(4):
                                tt = q * 4 + kk
                                r = e.transpose(out=PS[q][:, kk * 128:(kk + 1) * 128], in_=SL[i][:, tt * 128:(tt + 1) * 128],
                                                identity=ident32[:])
                            return r
                        P.op("pe", tr, reads=[nm("T", "nsl", i), CN("ident32")], writes=[BK(q)])
                        copy_op(evac_eng(), ys[:, q * 4:(q + 1) * 4, :], PS[q][:].rearrange("p (a b) -> p a b", b=128),
                                [BK(q)], [nm("OG", "ys", cg % 2, q)])
                    t = P.op("sp", lambda e, ys=ys, cg=cg: e.dma_start(
                        out=y_d[:, cg * 128:(cg + 1) * 128].rearrange("(tt p) f -> p tt f", p=128), in_=ys[:]),
                        reads=[nm("OG", "ys", cg % 2, q) for q in range(4)], writes=[nm("DR", "y", cg)],
                        dma="S_ys%d" % (cg % 2))
                    final.append(t)
        return [Job([], fn)]

    def fm_steps(wv, wname, nk, M, act_ap, act_names, epilogue, nb=4):
        steps = []
        for tc in range(NTC):
            def st(tc=tc):
                b = next_bank(nb, 0)
                mm_group(PS[b][0:M, :], M, wv, nk, lambda kc, tc=tc: act_ap[:, kc, tc * 512:(tc + 1) * 512],
                         [wname] + act_names, b)
                epilogue(b, tc)
            steps.append(st)
        return steps

    def fm_job(load, M, act_ap, act_names, epilogue, banks=(0, 4)):
        def fn(views, wn):
            for st in fm_steps(views[0], wn[0], load[1], M, act_ap, act_names, epilogue, banks[1]):
                st()
        return Job([load], fn)

    def tm_steps(wv, wname, dst_fn, dst_names_fn, nb=4):
        steps = []
        for g in range(4):
            def st(g=g):
                b = next_bank(nb, 0)

                def mm(e, g=g, b=b):
                    r = None
                    for k in range(4):
                        tt = g * 4 + k
                        for kc in range(KC):
                            r = e.matmul(PS[b][:, k * 128:(k + 1) * 128], lhsT=hT[:, kc, tt * 128:(tt + 1) * 128],
                                         rhs=wv[:, kc, :], start=(kc == 0), stop=(kc == KC - 1))
                    return r
                P.op("pe", mm, reads=[wname] + HN, writes=[BK(b)])
                copy_op(evac_eng(), dst_fn(g), PS[b][:].rearrange("p (a b) -> p a b", b=128), [BK(b)], dst_names_fn(g))
            steps.append(st)
        return steps

    def interleave(gen, steps, first_n, per):
        k = 0
        for i, _ in enumerate(gen):
            n = first_n if i == 0 else per
            for _ in range(n):
                if k < len(steps):
                    steps[k]()
                    k += 1
        while k < len(steps):
            steps[k]()
            k += 1

    def gla_jobs(l):
        out = []
        glrT = sb(R_OM + 0, [128, S], BF16)
        qT = sb(R_OM + 4096, [128, S], BF16)
        kT = sb(R_OM + 8192, [128, S], BF16)
        vhs = [sb(R_OM + 12288, [128, 16, 256], BF16), sb(R_T + 0, [128, 16, 256], BF16)]
        grSs = [sb(R_OM + 20480, [128, 2, S], BF16), sb(R_T + 8192, [128, 2, S], BF16)]
        Lbs = [sb(R_OM + 12288, [128, S], F32), sb(R_T + 0, [128, S], F32)]
        Cbs = [sb(R_OM + 20480, [128, S], F32), sb(R_T + 8192, [128, S], F32)]
        VA = [nm("OM", "va", 0), nm("T", "va", 1)]
        VB = [nm("OM", "vb", 0), nm("T", "vb", 1)]
        qt = sb(R_OM + 28672, [128, S], BF16)
        Eb = sb(R_T + 16384, [128, S], F32)
        kt = sb(R_T + 24576, [128, S], BF16)
        elast = sb(R_T + 28672, [128, 16], F32)
        attn_bf = [sb(R_T + 28800 + 256 * i, [128, 128], BF16) for i in range(2)]
        ktok = [sb(R_T + 29312 + 256 * i, [128, 128], BF16) for i in range(2)]
        state = sb(R_T + 29824, [128, 256], F32)
        tmpst = sb(R_T + 30848, [128, 256], F32)
        state_bf = [sb(R_T + 31872 + 512 * i, [128, 256], BF16) for i in range(2)]
        sqj = sb(R_T + 32896, [128, 256], F32)
        onb = [sb(R_T + 33920 + 512 * i, [128, 256], BF16) for i in range(2)]
        ssb = sb(R_T + 34944, [128, 4], F32)
        tmpexp = sb(R_T + 35008, [128, 512], F32)
        TN = lambda *k: nm("T", *k)
        MN = lambda *k: nm("OM", *k)

        def first(views, wn):
            P.fence("T")
            P.fence("OM")
            P.fence("OG")
        out.append(Job([], first))

        def ep_glr(b, tc):
            copy_op(evac_eng(), glrT[0:16, tc * 512:(tc + 1) * 512], PS[b][0:16, :], [BK(b)], [MN("glr", tc)])
        out.append(fm_job(wblock(w_in, l, 0, KC, OFF_GLR, 16), 16, hT, HN, ep_glr))

        def head_loads(h):
            return [wblock(w_in, l, 0, KC, OFF_GQ + h * 128, 128), wblock(w_in, l, 0, KC, OFF_GK + h * 128, 128),
                    wblock(w_in, l, 0, KC, OFF_GV + h * 256, 128), wblock(w_in, l, 0, KC, OFF_GR + h * 256, 128),
                    wblock(w_in, l, 0, KC, OFF_GV + h * 256 + 128, 128), wblock(w_in, l, 0, KC, OFF_GR + h * 256 + 128, 128)]

        def head_steps(h, views, wn):
            p = h % 2
            vh, grS = vhs[p], grSs[p]

            def ep_q(b, tc):
                copy_op(evac_eng(), qT[:, tc * 512:(tc + 1) * 512], PS[b][:], [BK(b)], [MN("q", tc)])

            def ep_k(b, tc):
                copy_op(evac_eng(), kT[:, tc * 512:(tc + 1) * 512], PS[b][:], [BK(b)], [MN("k", tc)])
            steps = fm_steps(views[0], wn[0], KC, 128, hT, HN, ep_q, 3) + fm_steps(views[1], wn[1], KC, 128, hT, HN, ep_k, 3)
            for half in range(2):
                steps += tm_steps(views[2 + 2 * half], wn[2 + 2 * half],
                                  lambda g, half=half: vh[:, g * 4:(g + 1) * 4, half * 128:(half + 1) * 128],
                                  lambda g: [VA[p]], 3)

                def ep_gr(b, tc, half=half):
                    P.op("act", lambda e: e.activation(out=grS[:, half, tc * 512:(tc + 1) * 512], in_=PS[b][:], func=AF.Silu),
                         reads=[BK(b)], writes=[VB[p]])
                steps += fm_steps(views[3 + 2 * half], wn[3 + 2 * half], KC, 128, hT, HN, ep_gr, 3)
            return steps

        def job0(views, wn):
            for st in head_steps(0, views, wn):
                st()
        out.append(Job(head_loads(0), job0))

        for h in range(4):
            def mixer(h=h):
                p = h % 2
                pp = (h + 1) % 2
                vh, grS, Lb, Cb = vhs[p], grSs[p], Lbs[pp], Cbs[pp]
                for tc in range(NTC):
                    b = next_bank()
                    P.op("pe", lambda e, b=b, tc=tc: e.matmul(PS[b][:], lhsT=gw2_bf[0:16, l, h * 128:(h + 1) * 128],
                                                              rhs=glrT[0:16, tc * 512:(tc + 1) * 512], start=True, stop=True),
                         reads=[CN("gw2_bf"), MN("glr", tc)], writes=[BK(b)])
                    P.op("act", lambda e, b=b: e.activation(out=tmpexp[:], in_=PS[b][:], func=AF.Exp, scale=-1.0,
                                                            bias=ngateb[:, l, h:h + 1]),
                         reads=[BK(b), CN("ngateb%d" % l)], writes=[TN("tmpexp")])
                    P.op("act", lambda e, tc=tc: e.activation(out=Lb[:, tc * 512:(tc + 1) * 512], in_=tmpexp[:], func=AF.Ln, bias=1.0),
                         reads=[TN("tmpexp")], writes=[VA[pp]])
                for c in range(16):
                    P.op("dve", lambda e, c=c: e.tensor_tensor_scan(out=Cb[:, c * 128:(c + 1) * 128], data0=ones32[:],
                                                                    data1=Lb[:, c * 128:(c + 1) * 128], initial=0.0,
                                                                    op0=ALU.mult, op1=ALU.add),
                         reads=[VA[pp], CN("ones32")], writes=[VB[pp]])
                CNs = [VB[pp]]
                P.op("act", lambda e: e.activation(out=Eb[:], in_=Cb[:], func=AF.Exp, scale=-1.0 / 16), reads=CNs, writes=[TN("E")])
                P.op("dve", lambda e: e.scalar_tensor_tensor(out=qt[:], in0=Eb[:], scalar=128.0 ** -0.5, in1=qT[:],
                                                             op0=ALU.mult, op1=ALU.mult),
                     reads=[TN("E")] + [MN("q", tc) for tc in range(NTC)], writes=[MN("qt")])
                P.op("act", lambda e: e.activation(out=elast[:], in_=Cb[:].rearrange("p (c t) -> p c t", t=128)[:, :, 127],
                                                   func=AF.Exp, scale=-1.0 / 16), reads=CNs, writes=[TN("elast")])
                P.op("act", lambda e: e.activation(out=Eb[:], in_=Cb[:], func=AF.Exp, scale=1.0 / 16),
                     reads=CNs + [TN("E")], writes=[TN("E")])
                P.op("dve", lambda e: e.tensor_tensor(out=kt[:], in0=Eb[:], in1=kT[:], op=ALU.mult),
                     reads=[TN("E")] + [MN("k", tc) for tc in range(NTC)], writes=[TN("kt")])
                yield
                B5b = PS[5][:].bitcast(BF16)
                vnames = [VA[p]]

                def stage_a(c):
                    ch = slice(c * 128, (c + 1) * 128)
                    i2 = c % 2
                    P.op("pe", lambda e: e.matmul(PS[4][:, 0:128], lhsT=kt[:, ch], rhs=qt[:, ch], start=True, stop=True),
                         reads=[TN("kt"), MN("qt")], writes=[BK(4)])
                    P.op("dve", lambda e: e.tensor_tensor(out=attn_bf[i2][:], in0=PS[4][:, 0:128], in1=maskU32[:], op=ALU.mult),
                         reads=[BK(4), CN("maskU32")], writes=[TN("attn", i2)])
                    if c < 15:
                        P.op("pe", lambda e: e.transpose(out=B5b[:, 0:128], in_=kt[:, ch], identity=ident_bf[:]),
                             reads=[TN("kt"), CN("ident_bf")], writes=[BK(5)])
                        P.op("act", lambda e: e.copy(out=ktok[i2][:], in_=B5b[:, 0:128]), reads=[BK(5)], writes=[TN("ktok", i2)])

                def stage_b(c):
                    ch = slice(c * 128, (c + 1) * 128)
                    i2 = c % 2
                    ob = 6 if c % 2 == 0 else 3

                    def omm(e):
                        r = e.matmul(PS[ob][:, 0:256], lhsT=attn_bf[i2][:], rhs=vh[:, c, :], start=True, stop=(c == 0))
                        if c > 0:
                            r = e.matmul(PS[ob][:, 0:256], lhsT=qt[:, ch], rhs=state_bf[(c - 1) % 2][:], start=False, stop=True)
                        return r
                    P.op("pe", omm, reads=[TN("attn", i2), MN("qt")] + vnames + ([TN("sbf", (c - 1) % 2)] if c > 0 else []),
                         writes=[BK(ob)])
                    if c < 15:
                        P.op("pe", lambda e: e.matmul(PS[7][:, 0:256], lhsT=ktok[i2][:], rhs=vh[:, c, :], start=True, stop=True),
                             reads=[TN("ktok", i2)] + vnames, writes=[BK(7)])
                        if c == 0:
                            P.op("dve", lambda e: e.tensor_copy(out=tmpst[:], in_=PS[7][:, 0:256]), reads=[BK(7)], writes=[TN("tmpst")])
                        else:
                            P.op("dve", lambda e: e.tensor_tensor(out=tmpst[:], in0=state[:], in1=PS[7][:, 0:256], op=ALU.add),
                                 reads=[BK(7), TN("state")], writes=[TN("tmpst")])
                        P.op("act", lambda e: e.activation(out=state_bf[c % 2][:], in_=tmpst[:], func=AF.Copy, scale=elast[:, c:c + 1]),
                             reads=[TN("tmpst"), TN("elast")], writes=[TN("sbf", c % 2)])
                        P.op("dve", lambda e: e.tensor_scalar(out=state[:], in0=tmpst[:], scalar1=elast[:, c:c + 1], scalar2=None, op0=ALU.mult),
                             reads=[TN("tmpst"), TN("elast")], writes=[TN("state")])
                    P.op("act", lambda e: e.activation(out=sqj[:], in_=PS[ob][:, 0:256], func=AF.Square, accum_out=ssb[:, 0:1]),
                         reads=[BK(ob)], writes=[TN("sqj"), TN("ss0")])
                    P.op("act", lambda e: e.activation(out=ssb[:, 1:2], in_=ssb[:, 0:1], func=AF.Sqrt, scale=1.0 / 256, bias=epsb[:, 0:1]),
                         reads=[TN("ss0"), CN("epsb")], writes=[TN("ss1")])
                    P.op("dve", lambda e: e.reciprocal(out=ssb[:, 2:3], in_=ssb[:, 1:2]), reads=[TN("ss1")], writes=[TN("ss2")])
                    P.op("dve", lambda e: e.scalar_tensor_tensor(out=onb[i2][:], in0=PS[ob][:, 0:256], scalar=ssb[:, 2:3],
                                                                 in1=gnormb[:, l, :], op0=ALU.mult, op1=ALU.mult),
                         reads=[BK(ob), TN("ss2"), CN("gnormb%d" % l)], writes=[TN("on", i2)])

                def stage_c(c):
                    ch = slice(c * 128, (c + 1) * 128)
                    i2 = c % 2

                    def otr(e):
                        e.transpose(out=B5b[:, 256:384], in_=onb[i2][:, 0:128], identity=ident_bf[:])
                        return e.transpose(out=B5b[:, 384:512], in_=onb[i2][:, 128:256], identity=ident_bf[:])
                    P.op("pe", otr, reads=[TN("on", i2), CN("ident_bf")], writes=[BK(5)])
                    P.op("dve", lambda e: e.tensor_tensor(
                        out=OG[:, 2 * h:2 * h + 2, ch], in0=B5b[:, 256:512].rearrange("p (a b) -> p a b", b=128),
                        in1=grS[:, :, ch], op=ALU.mult),
                        reads=[BK(5), VB[p]], writes=[nm("OG", "og", h, c)])

                stage_a(0)
                for c in range(16):
                    if c + 1 < 16:
                        stage_a(c + 1)
                    stage_b(c)
                    if c >= 1:
                        stage_c(c - 1)
                    yield
                stage_c(15)

            def run(views, wn, h=h, mixer=mixer):
                steps = head_steps(h + 1, views, wn) if h < 3 else []
                interleave(mixer(), steps, 8, 1)
            out.append(Job(head_loads(h + 1) if h < 3 else [], run))
        return out

    def moba_jobs(l):
        out = []
        qTs = [sb(R_T + 12416 * p + 0, [128, S], BF16) for p in range(2)]
        kTs = [sb(R_T + 12416 * p + 4096, [128, S], BF16) for p in range(2)]
        v1s = [sb(R_T + 12416 * p + 8192, [128, 16, 132], BF16) for p in range(2)]
        O2 = 12416
        km32 = sb(R_T + O2 + 12416, [128, 8], F32)
        km_bf = sb(R_T + O2 + 12480, [128, 8], BF16)
        sc_all = sb(R_T + O2 + 12544, [128, 16, 8], F32)
        top8 = sb(R_T + O2 + 13056, [128, 8], F32)
        sel = sb(R_T + O2 + 13120, [128, 16, 8], F32)
        Pb = [sb(R_T + O2 + 13632 + 1024 * i, [128, 512], BF16) for i in range(4)]
        accb = [sb(R_T + O2 + 17728 + 544 * i, [128, 132], F32) for i in range(2)]
        rden = sb(R_T + O2 + 18816, [128, 2], F32)
        otok = sb(R_T + O2 + 18848, [128, 16, 128], BF16)
        TN = lambda *k: nm("T", *k)

        def first(views, wn):
            P.fence("T")
            P.fence("OM")
            for p in range(2):
                P.op("dve", lambda e, p=p: e.memset(v1s[p][:, :, 128:132], 1.0), writes=[TN("v1ones", p)])
        out.append(Job([], first))

        def head_loads(h):
            return [wblock(w_in, l, 0, KC, OFF_MQ + h * 128, 128), wblock(w_in, l, 0, KC, OFF_MK + h * 128, 128),
                    wblock(w_in, l, 0, KC, OFF_MV + h * 128, 128)]

        def head_steps(h, views, wn):
            p = h % 2
            qT, kT, v1 = qTs[p], kTs[p], v1s[p]

            def ep_q(b, tc):
                copy_op(evac_eng(), qT[:, tc * 512:(tc + 1) * 512], PS[b][:], [BK(b)], [TN("q", p, tc)], scale=128.0 ** -0.5)

            def ep_k(b, tc):
                copy_op(evac_eng(), kT[:, tc * 512:(tc + 1) * 512], PS[b][:], [BK(b)], [TN("k", p, tc)])
            return (fm_steps(views[0], wn[0], KC, 128, hT, HN, ep_q, 2) + fm_steps(views[1], wn[1], KC, 128, hT, HN, ep_k, 2)
                    + tm_steps(views[2], wn[2], lambda g: v1[:, g * 4:(g + 1) * 4, 0:128], lambda g: [TN("v", p, g)], 2))

        def job0(views, wn):
            for st in head_steps(0, views, wn):
                st()
        out.append(Job(head_loads(0), job0))

        for h in range(8):
            def mixer(h=h):
                p = h % 2
                qT, kT, v1 = qTs[p], kTs[p], v1s[p]
                QN = [TN("q", p, tc) for tc in range(NTC)]
                KN = [TN("k", p, tc) for tc in range(NTC)]
                P.op("dve", lambda e: e.tensor_reduce(out=km32[:], in_=kT[:].rearrange("p (b t) -> p b t", t=256), axis=AX.X, op=ALU.add),
                     reads=KN, writes=[TN("km32")])
                P.op("dve", lambda e: e.tensor_scalar(out=km_bf[:], in0=km32[:], scalar1=1.0 / 256, scalar2=None, op0=ALU.mult),
                     reads=[TN("km32")], writes=[TN("km")])

                def bs(e):
                    r = None
                    for tt in range(16):
                        r = e.matmul(PS[3][:, tt * 8:(tt + 1) * 8], lhsT=qT[:, tt * 128:(tt + 1) * 128], rhs=km_bf[:], start=True, stop=True)
                    return r
                P.op("pe", bs, reads=QN + [TN("km")], writes=[BK(3)])
                P.op("dve", lambda e: e.tensor_tensor(out=sc_all[:], in0=PS[3][:, 0:128].rearrange("p (a b) -> p a b", b=8),
                                                      in1=padmask[:], op=ALU.add),
                     reads=[BK(3), CN("padmask")], writes=[TN("sc")])
                for tt in range(8, 16):
                    P.op("dve", lambda e, tt=tt: e.max(out=top8[:], in_=sc_all[:, tt, :]), reads=[TN("sc")], writes=[TN("top8")])
                    P.op("dve", lambda e, tt=tt: e.tensor_scalar(out=sel[:, tt, :], in0=sc_all[:, tt, :], scalar1=top8[:, 2:3],
                                                                 scalar2=None, op0=ALU.is_ge),
                         reads=[TN("sc"), TN("top8")], writes=[TN("sel", tt)])
                prot = [0]
                srot = [0]
                yield
                obanks = [6, 7, 3]

                def oview(ot):
                    bk = obanks[ot // 3]
                    return PS[bk][:, (ot % 3) * 132:(ot % 3) * 132 + 129], bk
                items = []
                for tq in range(16):
                    i = tq // 2
                    past = list(range(0, 2 * i))
                    groups = [past[g:g + 4] for g in range(0, len(past), 4)]
                    diag = [2 * i] if tq % 2 == 0 else [2 * i, 2 * i + 1]
                    allg = groups + [diag]
                    for gi, g in enumerate(allg):
                        items.append(dict(tq=tq, i=i, gi=gi, g=g, is_diag=(gi == len(allg) - 1), sparse=(i >= 4),
                                          sb=[2, 4, 5][len(items) % 3], pi=len(items) % 4))

                def emit_score(it):
                    tq, g, sb_, pi = it["tq"], it["g"], it["sb"], it["pi"]
                    tqs = slice(tq * 128, (tq + 1) * 128)
                    qn = [TN("q", p, tq // 4)]

                    def smm(e):
                        r = None
                        for k, ts in enumerate(g):
                            r = e.matmul(PS[sb_][:, k * 128:(k + 1) * 128], lhsT=kT[:, ts * 128:(ts + 1) * 128], rhs=qT[:, tqs],
                                         start=True, stop=True)
                        return r
                    P.op("pe", smm, reads=qn + KN, writes=[BK(sb_)])
                    n = len(g)
                    P.op("act", lambda e: e.activation(out=Pb[pi][:, 0:n * 128], in_=PS[sb_][:, 0:n * 128], func=AF.Exp),
                         reads=[BK(sb_)], writes=[TN("P", pi)])
                    if it["is_diag"]:
                        kd = len(g) - 1
                        P.op("dve", lambda e: e.tensor_tensor(out=Pb[pi][:, kd * 128:(kd + 1) * 128],
                                                              in0=Pb[pi][:, kd * 128:(kd + 1) * 128],
                                                              in1=maskU_bf[:], op=ALU.mult),
                             reads=[TN("P", pi), CN("maskU_bf")], writes=[TN("P", pi)])

                def emit_pv(it):
                    tq, i, gi, g, pi, is_diag, sparse = it["tq"], it["i"], it["gi"], it["g"], it["pi"], it["is_diag"], it["sparse"]
                    vn = [TN("P", pi), TN("v1ones", p)]
                    if not sparse or is_diag:
                        def pv(e):
                            r = None
                            ov, _ = oview(0)
                            for k, ts in enumerate(g):
                                st_ = (k == 0) if sparse else (gi == 0 and k == 0)
                                r = e.matmul(ov, lhsT=Pb[pi][:, k * 128:(k + 1) * 128], rhs=v1[:, ts, 0:129],
                                             start=st_, stop=(is_diag and k == len(g) - 1))
                            return r
                        P.op("pe", pv, reads=vn + [TN("v", p, ts // 4) for ts in g], writes=[BK(6)])
                    else:
                        for jj in range(len(g) // 2):
                            j = g[2 * jj] // 2
                            ov, bk = oview(1 + j)

                            def pv(e, jj=jj, ov=ov):
                                e.matmul(ov, lhsT=Pb[pi][:, (2 * jj) * 128:(2 * jj + 1) * 128], rhs=v1[:, g[2 * jj], 0:129], start=True, stop=False)
                                return e.matmul(ov, lhsT=Pb[pi][:, (2 * jj + 1) * 128:(2 * jj + 2) * 128], rhs=v1[:, g[2 * jj + 1], 0:129],
                                                start=False, stop=True)
                            P.op("pe", pv, reads=vn + [TN("v", p, ts // 4) for ts in g[2 * jj:2 * jj + 2]], writes=[BK(bk)])

                def finalize(tq):
                    i = tq // 2
                    if i < 4:
                        ov, _ = oview(0)
                        P.op("dve", lambda e: e.reciprocal(out=rden[:, 0:1], in_=ov[:, 128:129]), reads=[BK(6)], writes=[TN("rden")])
                        P.op("dve", lambda e: e.tensor_scalar(out=otok[:, tq, :], in0=ov[:, 0:128], scalar1=rden[:, 0:1],
                                                              scalar2=None, op0=ALU.mult),
                             reads=[BK(6), TN("rden")], writes=[TN("otok", tq)])
                    else:
                        ai = tq % 2
                        ov, _ = oview(0)
                        P.op("dve", lambda e: e.tensor_copy(out=accb[ai][:, 0:129], in_=ov), reads=[BK(6)], writes=[TN("acc", ai)])
                        for j in range(i):
                            ovj, bk = oview(1 + j)
                            P.op("dve", lambda e, ovj=ovj, j=j: e.scalar_tensor_tensor(
                                out=accb[ai][:, 0:129], in0=ovj, scalar=sel[:, tq, j:j + 1], in1=accb[ai][:, 0:129],
                                op0=ALU.mult, op1=ALU.add),
                                reads=[BK(bk), TN("sel", tq), TN("acc", ai)], writes=[TN("acc", ai)])
                        P.op("dve", lambda e: e.reciprocal(out=rden[:, 0:1], in_=accb[ai][:, 128:129]), reads=[TN("acc", ai)], writes=[TN("rden")])
                        P.op("dve", lambda e: e.tensor_scalar(out=otok[:, tq, :], in0=accb[ai][:, 0:128], scalar1=rden[:, 0:1],
                                                              scalar2=None, op0=ALU.mult),
                             reads=[TN("acc", ai), TN("rden")], writes=[TN("otok", tq)])

                emit_score(items[0])
                emit_score(items[1])
                for n_, it in enumerate(items):
                    if n_ + 2 < len(items):
                        emit_score(items[n_ + 2])
                    emit_pv(it)
                    if it["is_diag"]:
                        finalize(it["tq"])
                        yield
                for g in range(4):
                    bk = 4 + g % 2
                    Bb = PS[bk][:].bitcast(BF16)

                    def tr(e, g=g, Bb=Bb):
                        r = None
                        for k in range(4):
                            r = e.transpose(out=Bb[:, k * 128:(k + 1) * 128], in_=otok[:, g * 4 + k, :], identity=ident_bf[:])
                        return r
                    P.op("pe", tr, reads=[TN("otok", g * 4 + k) for k in range(4)] + [CN("ident_bf")], writes=[BK(bk)])
                    copy_op(evac_eng(), OM[:, h, g * 512:(g + 1) * 512], Bb[:, 0:512], [BK(bk)], [nm("OM", "om", h, g)])

            def run(views, wn, h=h, mixer=mixer):
                steps = head_steps(h + 1, views, wn) if h < 7 else []
                interleave(mixer(), steps, 1, 1)
            out.append(Job(head_loads(h + 1) if h < 7 else [], run))
        return out

    def p5_jobs(l):
        out = []
        TN = lambda *k: nm("T", *k)
        tmp = [[sb(R_T + 8192 * s + 2048 * k, [128, 512], F32) for k in range(4)] for s in range(2)]
        MTS = [sb(R_T + 16384 + 4096 * i, [128, S], BF16) for i in range(2)]
        OGN = [nm("OG", "og", h, c) for h in range(4) for c in range(16)]
        OMN = [nm("OM", "om", h, g) for h in range(8) for g in range(4)]

        def first(views, wn):
            P.fence("T")
        out.append(Job([], first))
        cnt = [0]
        for fc in range(KC):
            def fn(views, wn, fc=fc):
                wug, wum, wgg, wgm = views
                ms = MTS[fc % 2]
                for tc in range(NTC):
                    s = cnt[0] % 2
                    cnt[0] += 1
                    bs_ = [4 * s + k for k in range(4)]
                    tsl = slice(tc * 512, (tc + 1) * 512)
                    mm_group(PS[bs_[0]][:], 128, wug, 8, lambda kc, tsl=tsl: OG[:, kc, tsl], [wn[0]] + OGN, bs_[0])
                    mm_group(PS[bs_[1]][:], 128, wum, 8, lambda kc, tsl=tsl: OM[:, kc, tsl], [wn[1]] + OMN, bs_[1])
                    mm_group(PS[bs_[2]][:], 128, wgg, KC, lambda kc, tsl=tsl: hT[:, kc, tsl], [wn[2]] + HN, bs_[2])
                    mm_group(PS[bs_[3]][:], 128, wgm, KC, lambda kc, tsl=tsl: hT[:, kc, tsl], [wn[3]] + HN, bs_[3])
                    sg, sm, t1, t2 = tmp[s]
                    P.op("act", lambda e, sg=sg, b=bs_[2]: e.activation(out=sg[:], in_=PS[b][:], func=AF.Sigmoid), reads=[BK(bs_[2])], writes=[TN("sg", s)])
                    P.op("act", lambda e, sm=sm, b=bs_[3]: e.activation(out=sm[:], in_=PS[b][:], func=AF.Sigmoid), reads=[BK(bs_[3])], writes=[TN("sm", s)])
                    P.op("dve", lambda e, t1=t1, sg=sg, b=bs_[0]: e.tensor_tensor(out=t1[:], in0=PS[b][:], in1=sg[:], op=ALU.mult),
                         reads=[BK(bs_[0]), TN("sg", s)], writes=[TN("t1", s)])
                    P.op("dve", lambda e, t2=t2, sm=sm, b=bs_[1]: e.tensor_tensor(out=t2[:], in0=PS[b][:], in1=sm[:], op=ALU.mult),
                         reads=[BK(bs_[1]), TN("sm", s)], writes=[TN("t2", s)])
                    P.op("dve", lambda e, t1=t1, t2=t2, ms=ms, tsl=tsl: e.tensor_tensor(out=ms[:, tsl], in0=t1[:], in1=t2[:], op=ALU.add),
                         reads=[TN("t1", s), TN("t2", s)], writes=[TN("mts", fc % 2, tc)])
                P.op("sp", lambda e, ms=ms: e.dma_start(out=MT_d[fc], in_=ms[:]),
                     reads=[TN("mts", fc % 2, tc) for tc in range(NTC)], writes=[nm("DR", "MT", fc)], dma="S_mts%d" % (fc % 2))
            loads = [wblock(w_up_gla, l, 0, 8, fc * 128, 128), wblock(w_up_moba, l, 0, 8, fc * 128, 128),
                     wblock(w_in, l, 0, KC, OFF_GG + fc * 128, 128), wblock(w_in, l, 0, KC, OFF_GM + fc * 128, 128)]
            out.append(Job(loads, fn))
        return out

    class RMW:
        def __init__(self, off, n=8, sq_off=None):
            self.tiles = [sb(off + 2048 * i, [128, 512], F32) for i in range(n)]
            self.sq = [sb(sq_off + 1024 * i, [128, 512], BF16) for i in range(4)] if sq_off is not None else None
            self.n = n
            self.k_load = 0
            self.k_use = 0
            self.order = []
            self.pending = []

        def plan(self, items):
            self.order = list(items)

        def prefetch(self, upto):
            while self.k_load < min(upto, len(self.order)):
                cg, tc, _ = self.order[self.k_load]
                i = self.k_load % self.n
                P.op("sp", lambda e, i=i, cg=cg, tc=tc: e.dma_start(out=self.tiles[i][:], in_=R_d[cg, :, tc * 512:(tc + 1) * 512]),
                     reads=[nm("DR", "R", cg, tc)], writes=[nm("T", "rt", i)], dma="S_rt%d" % i)
                self.k_load += 1

        def apply(self, bank, gate_ap, gate_names):
            k = self.k_use
            self.k_use += 1
            cg, tc, st = self.order[k]
            i = k % self.n
            self.prefetch(k + 1)
            P.op("dve", lambda e, i=i: e.scalar_tensor_tensor(out=self.tiles[i][:], in0=PS[bank][:], scalar=gate_ap,
                                                              in1=self.tiles[i][:], op0=ALU.mult, op1=ALU.add),
                 reads=[BK(bank), nm("T", "rt", i)] + gate_names, writes=[nm("T", "rt", i)])
            P.op("sp", lambda e, i=i, cg=cg, tc=tc: e.dma_start(out=R_d[cg, :, tc * 512:(tc + 1) * 512], in_=self.tiles[i][:]),
                 reads=[nm("T", "rt", i)], writes=[nm("DR", "R", cg, tc)], dma="S_rs%d" % i)
            if st:
                j = k % 4
                P.op("act", lambda e, i=i, j=j: e.activation(out=self.sq[j][:], in_=self.tiles[i][:], func=AF.Square),
                     reads=[nm("T", "rt", i)], writes=[nm("T", "rsq", j)])
                self.pending.append((j, tc, cg))
            last = (k == len(self.order) - 1)
            while self.pending and (len(self.pending) > 2 or last):
                j, tc2, cg2 = self.pending.pop(0)
                P.op("pe", lambda e, j=j, tc2=tc2, cg2=cg2: e.matmul(PS[4 + tc2][:], lhsT=ones_bf[:], rhs=self.sq[j][:],
                                                                     start=(cg2 == 0), stop=(cg2 == KC - 1)),
                     reads=[nm("T", "rsq", j), CN("ones_bf")], writes=[BK(4 + tc2)])
            self.prefetch(k + self.n - 1)

    def p6_jobs(l):
        out = []
        MTN = [nm("A", "mt", fc) for fc in range(KC)]
        rmw = [None]

        def first(views, wn):
            P.fence("T")
            P.fence("A")
            tk = None
            for fc in range(KC):
                tk = P.op("sp", lambda e, fc=fc: e.dma_start(out=hT[:, fc, :], in_=MT_d[fc]), reads=[nm("DR", "MT", fc)],
                          writes=[MTN[fc]], dma="S_mtl")
            for fc in range(KC):
                P.lastw[MTN[fc]] = tk
            rmw[0] = RMW(R_T + 0, sq_off=R_T + 16384)
            rmw[0].plan([(cg, tc, True) for cg in range(KC) for tc in range(NTC)])
            rmw[0].prefetch(7)
        out.append(Job([], first))
        for cg in range(KC):
            def ep(b, tc, cg=cg):
                rmw[0].apply(b, modv[:, l, 32 + cg:32 + cg + 1], [CN("mod%d_%d" % (l, 32))])
            out.append(fm_job(wblock(w_out, l, 0, KC, cg * 128, 128), 128, hT, MTN, ep))
        return out

    def ffn_jobs(l, extra=None):
        out = []
        TN = lambda *k: nm("T", *k)
        bounds = [round(q * NJ / NQ) for q in range(NQ + 1)]
        sa = [sb(R_T + 2048 * i, [128, 512], F32) for i in range(2)]
        rmw = [None]
        cnt = [0]

        def first(views, wn):
            P.fence("T")
            P.fence("OG")
            P.fence("OM")
            rmw[0] = RMW(R_T + 4096, sq_off=R_T + 20480)
            rmw[0].plan([(cg, tc, q == NQ - 1) for q in range(NQ) for cg in range(KC) for tc in range(NTC)])
        out.append(Job([], first))
        for q in range(NQ):
            j0, j1 = bounds[q], bounds[q + 1]
            nj = j1 - j0
            for j in range(j0, j1):
                def fn(views, wn, j=j, j0=j0):
                    wa, wu = views
                    jj = j - j0
                    for tc in range(NTC):
                        s = cnt[0] % 2
                        cnt[0] += 1
                        ba, bu = 2 * s, 2 * s + 1
                        tsl = slice(tc * 512, (tc + 1) * 512)
                        mm_group(PS[ba][:], 128, wa, KC, lambda kc, tsl=tsl: hT[:, kc, tsl], [wn[0]] + HN, ba)
                        mm_group(PS[bu][:], 128, wu, KC, lambda kc, tsl=tsl: hT[:, kc, tsl], [wn[1]] + HN, bu)
                        P.op("act", lambda e, s=s, ba=ba: e.activation(out=sa[s][:], in_=PS[ba][:], func=AF.Silu), reads=[BK(ba)], writes=[TN("sa", s)])
                        P.op("dve", lambda e, s=s, bu=bu, jj=jj, tsl=tsl: e.tensor_tensor(out=FF[:, jj, tsl], in0=PS[bu][:], in1=sa[s][:], op=ALU.mult),
                             reads=[BK(bu), TN("sa", s)], writes=[nm(("OG", "OM"), "ff", jj, tc)])
                out.append(Job([wblock(w_ffn_in, l, 0, KC, j * 128, 128), wblock(w_ffn_in, l, 0, KC, DFF + j * 128, 128)], fn))
                if extra:
                    out.append(extra.pop(0))
            for cg in range(KC):
                def fn2(views, wn, cg=cg, nj=nj, q=q):
                    wv = views[0]
                    if q == 0 and cg == 0:
                        rmw[0].prefetch(7)
                    for tc in range(NTC):
                        b = next_bank(4, 0) if q == NQ - 1 else 4 + next_bank(2, 0)
                        tsl = slice(tc * 512, (tc + 1) * 512)
                        mm_group(PS[b][:], 128, wv, nj, lambda kc, tsl=tsl: FF[:, kc, tsl],
                                 [wn[0]] + [nm(("OG", "OM"), "ff", jj, tc) for jj in range(nj)], b)
                        rmw[0].apply(b, modv[:, l, 80 + cg:80 + cg + 1], [CN("mod%d_%d" % (l, 80))])
                out.append(Job([wblock(w_ffn_out, l, j0 * 128, nj, cg * 128, 128)], fn2))
                if extra:
                    out.append(extra.pop(0))
            if q < NQ - 1:
                def fq(views, wn):
                    P.fence("OG")
                    P.fence("OM")
                out.append(Job([], fq))
        return out

    setup()

    def ada_list(l, j0, j1):
        return [ada_job(l, j) for j in range(j0, j1)]

    STAGES = ["mod", "x0", "n1", "gla", "moba", "p5", "p6", "n2", "ffn", "final"]

    def stage_idx(s):
        return STAGES.index(s)
    stop_i = stage_idx(stop) if stop else 10 ** 6
    nlayers = DEPTH
    def mix(main, extra, per):
        res = []
        extra = list(extra)
        for jb in main:
            res.append(jb)
            for _ in range(per):
                if extra:
                    res.append(extra.pop(0))
        return res + extra

    for l in range(nlayers):
        deferred = []
        if l == 0:
            early = ada_list(0, 0, 32) + [gmod_job(0, 1)]
            jobs += mix(x0_jobs(), early, 2)
            deferred = ada_list(0, 32, 96) + [gmod_job(0, 2)]
        jobs += norm_jobs(l, 1)
        if l == 0 and stop_i == stage_idx("n1"):
            break
        jobs += mix(gla_jobs(l), deferred[:14], 2)
        if l == 0 and stop_i == stage_idx("gla"):
            break
        jobs += mix(moba_jobs(l), deferred[14:34], 2)
        if l == 0 and stop_i == stage_idx("moba"):
            break
        jobs += mix(p5_jobs(l), deferred[34:], 2)
        if l == 0 and stop_i == stage_idx("p5"):
            break
        jobs += p6_jobs(l)
        if l == 0 and stop_i == stage_idx("p6"):
            break
        jobs += norm_jobs(l, 2)
        if l == 0 and stop_i == stage_idx("n2"):
            break
        extra = None
        if l == 0:
            extra = ada_list(1, 0, 96) + [gmod_job(1, 1), gmod_job(1, 2)]
        jobs += ffn_jobs(l, extra)
        if extra:
            jobs += extra
        if l == 0 and stop_i == stage_idx("ffn"):
            break
    else:
        jobs += norm_jobs(0, 3)

    load_q = []
    for ji, job in enumerate(jobs):
        for li in range(len(job.loads)):
            load_q.append((ji, li))
    issued = [0]

    def slot_view(k, nk, ncols):
        return WS[k % NSLOT][:, 0:nk * ncols].rearrange("p (k c) -> p k c", c=ncols)

    def issue_upto(n):
        while issued[0] < min(n, len(load_q)):
            k = issued[0]
            ji, li = load_q[k]
            ap, nk, ncols = jobs[ji].loads[li]
            dst = slot_view(k, nk, ncols)
            P.op("pool", lambda e, dst=dst, ap=ap: e.dma_start(out=dst, in_=ap), writes=[nm("W", k % NSLOT)],
                 dma="S_w%d" % (k % NSLOT))
            issued[0] += 1
    ptr = 0
    for ji, job in enumerate(jobs):
        nl = len(job.loads)
        issue_upto(ptr + NSLOT)
        views = [slot_view(ptr + li, job.loads[li][1], job.loads[li][2]) for li in range(nl)]
        wn = [nm("W", (ptr + li) % NSLOT) for li in range(nl)]
        job.fn(views, wn)
        ptr += nl

    dump("hT", hT[:], [128, KC, S], BF16, HN)
    dump("modv", modv[:], [128, 2, 96], F32, [CN("mod%d_%d" % (l, j)) for l in range(1) for j in range(0, 96, 16)])
    dump("OG", OG[:], [128, 8, S], BF16, [nm("OG", "og", h, c) for h in range(4) for c in range(16)])
    dump("OM", OM[:], [128, 8, S], BF16, [nm("OM", "om", h, g) for h in range(8) for g in range(4)])
    if "R" in dbg_names:
        d = nc.dram_tensor("dbg_R", [KC, 128, S], F32, kind="ExternalOutput").ap()
        for cg in range(KC):
            t = P.op("sp", lambda e, d=d, cg=cg: e.dma_start(out=d[cg], in_=R_d[cg]),
                     reads=[nm("DR", "R", cg, tc) for tc in range(NTC)], writes=[nm("DR", "dbgR", cg)], dma="S_dbg")
        final.append(t)
    if "MT" in dbg_names:
        d = nc.dram_tensor("dbg_MT", [KC, 128, S], BF16, kind="ExternalOutput").ap()
        for fc in range(KC):
            t = P.op("sp", lambda e, d=d, fc=fc: e.dma_start(out=d[fc], in_=MT_d[fc]), reads=[nm("DR", "MT", fc)],
                     writes=[nm("DR", "dbgMT", fc)], dma="S_dbg")
        final.append(t)
    if not final:
        pass
    P.emit(final)
    return nc


_NC_CACHE = {}


def kernel(x, c, ada_w, ada_b, norm1_g, w_in, gla_gate_w2, gla_gate_b, gla_norm_g, w_up_gla, w_up_moba, w_out,
           norm2_g, w_ffn_in, w_ffn_out, final_g):
    f = lambda a: np.ascontiguousarray(np.asarray(a, dtype=np.float32))
    x = f(x)
    c = f(c)
    shared = dict(ada_w=f(ada_w), ada_b=f(ada_b), norm1_g=f(norm1_g), w_in=f(w_in), gla_gate_w2=f(gla_gate_w2),
                  gla_gate_b=f(gla_gate_b), gla_norm_g=f(gla_norm_g), w_up_gla=f(w_up_gla), w_up_moba=f(w_up_moba),
                  w_out=f(w_out), norm2_g=f(norm2_g), w_ffn_in=f(w_ffn_in), w_ffn_out=f(w_ffn_out), final_g=f(final_g))
    nc = build_program()
    in_maps = []
    for b in range(8):
        m = dict(shared)
        m["x"] = x[b]
        m["c"] = c[b]
        in_maps.append(m)
    res = run_bass_kernel_spmd(nc, in_maps, core_ids=list(range(8)))
    return np.stack([np.asarray(r["y"], dtype=np.float32) for r in res.results], axis=0)
```

```python
import contextlib
import numpy as np
import concourse.bass as bass
import concourse.mybir as mybir
from concourse.alu_op_type import AluOpType as ALU
from concourse.bass_utils import run_bass_kernel_spmd

F32 = mybir.dt.float32
BF16 = mybir.dt.bfloat16
AF = mybir.ActivationFunctionType
AX = mybir.AxisListType

D = 2048
S = 2048
KC = 16
NTC = 4
NTT = 16
DFF = 5632
NJ = 44
DEPTH = 2
EPS = 1e-6
OFF_GQ, OFF_GK, OFF_GV, OFF_GR, OFF_GLR = 0, 512, 1024, 2048, 3072
OFF_MQ, OFF_MK, OFF_MV, OFF_GG, OFF_GM = 3088, 4112, 5136, 6160, 8208
NQ = 3
FUSED_STATS = True

BASE = 16512
R_CONST, R_A, R_OG, R_OM, R_W, R_T = 0, 10240, 75776, 108544, 141312, 165888
NSLOT = 6


def nm(regions, *key):
    if isinstance(regions, str):
        regions = (regions,)
    return (tuple(regions),) + tuple(key)


class Prog:
    ENG = ("pe", "act", "dve", "pool", "sp")

    def __init__(self, nc):
        self.nc = nc
        self.items = {e: [] for e in self.ENG}
        self.cnt = {e: 0 for e in self.ENG}
        self.sems = {}
        self.lastw = {}
        self.readers = {}
        self.waited = {e: {} for e in self.ENG}
        self.guard = {}

    def fence(self, region):
        g = self.guard.setdefault(region, {})
        names = [n for n in set(self.lastw) | set(self.readers) if region in n[0]]
        for n in names:
            toks = []
            t = self.lastw.pop(n, None)
            if t is not None:
                toks.append(t)
            toks.extend(self.readers.pop(n, ()))
            for s, v in toks:
                if g.get(s, 0) < v:
                    g[s] = v

    def op(self, eng, fn, reads=(), writes=(), dma=None):
        need = {}

        def add(t):
            if t is not None and need.get(t[0], 0) < t[1]:
                need[t[0]] = t[1]

        for r in reads:
            add(self.lastw.get(r))
        for w in writes:
            if w not in self.lastw:
                for reg in w[0]:
                    for s, v in self.guard.get(reg, {}).items():
                        add((s, v))
            add(self.lastw.get(w))
            for t in self.readers.get(w, ()):
                add(t)
        wl = []
        for s, v in need.items():
            if self.waited[eng].get(s, 0) < v:
                self.waited[eng][s] = v
                wl.append((s, v))
        if dma is None:
            self.cnt[eng] += 1
            tok = ("E_" + eng, self.cnt[eng])
            inc = (tok[0], 1)
        else:
            self.sems[dma] = self.sems.get(dma, 0) + 16
            tok = (dma, self.sems[dma])
            inc = (dma, 16)
        self.items[eng].append((wl, fn, inc))
        for r in reads:
            self.readers.setdefault(r, []).append(tok)
        for w in writes:
            self.lastw[w] = tok
            self.readers[w] = []
        return tok

    def emit(self, final_tokens):
        nc = self.nc
        names = set("E_" + e for e in self.ENG)
        names.update(self.sems.keys())
        names = sorted(names)
        with contextlib.ExitStack() as st:
            semo = {n: st.enter_context(nc.semaphore(n)) for n in names}
            block = st.enter_context(nc.Block())
            engmap = {"pe": "tensor", "act": "scalar", "dve": "vector", "pool": "gpsimd", "sp": "sync"}

            def mk(e):
                def body(eng):
                    for (wl, fn, inc) in self.items[e]:
                        for s, v in wl:
                            eng.wait_ge(semo[s], v)
                        inst = fn(eng)
                        inst.then_inc(semo[inc[0]], inc[1])
                    if e == "sp":
                        for (s, v) in final_tokens:
                            eng.wait_ge(semo[s], v)
                return body

            for e in self.ENG:
                if self.items[e] or e == "sp":
                    getattr(block, engmap[e])(mk(e))


class Job:
    def __init__(self, loads, fn):
        self.loads = loads
        self.fn = fn


def build_program(stop=None, dbg_names=()):
    nc = bass.Bass("TRN2", target_bir_lowering=False)
    P = Prog(nc)

    def din(name, shape):
        return nc.dram_tensor(name, list(shape), F32, kind="ExternalInput").ap()

    x_d = din("x", [S, D])
    c_d = din("c", [D])
    ada_w = din("ada_w", [DEPTH, D, 6 * D])
    ada_b = din("ada_b", [DEPTH, 6 * D])
    norm1_g = din("norm1_g", [DEPTH, D])
    w_in = din("w_in", [DEPTH, D, 10256])
    gate_w2 = din("gla_gate_w2", [DEPTH, 16, 512])
    gate_b = din("gla_gate_b", [DEPTH, 512])
    gnorm_g = din("gla_norm_g", [DEPTH, 256])
    w_up_gla = din("w_up_gla", [DEPTH, 1024, D])
    w_up_moba = din("w_up_moba", [DEPTH, 1024, D])
    w_out = din("w_out", [DEPTH, D, D])
    norm2_g = din("norm2_g", [DEPTH, D])
    w_ffn_in = din("w_ffn_in", [DEPTH, D, 2 * DFF])
    w_ffn_out = din("w_ffn_out", [DEPTH, DFF, D])
    final_g = din("final_g", [D])
    y_d = nc.dram_tensor("y", [S, D], F32, kind="ExternalOutput").ap()
    R_d = nc.dram_tensor("resid", [KC, 128, S], F32, kind="Internal").ap()
    MT_d = nc.dram_tensor("mt_scr", [KC, 128, S], BF16, kind="Internal").ap()
    dbg_out = {}

    _cnt = [0]

    def sb(off, shape, dt, name=None):
        _cnt[0] += 1
        return nc.alloc_sbuf_tensor_at("%s_%d" % (name or "t", _cnt[0]), list(shape), dt, offset=BASE + off)

    PS = [nc.alloc_psum_tensor("B%d" % i, [128, 512], F32) for i in range(8)]

    def BK(b):
        return nm("PS", b)

    ident_bf = sb(R_CONST + 0, [128, 128], BF16)
    ident32 = sb(R_CONST + 256, [128, 128], F32)
    ones_bf = sb(R_CONST + 768, [128, 128], BF16)
    ones32 = sb(R_CONST + 1024, [128, 128], F32)
    maskU32 = sb(R_CONST + 1536, [128, 128], F32)
    maskU_bf = sb(R_CONST + 2048, [128, 128], BF16)
    padmask = sb(R_CONST + 2304, [128, 16, 8], F32)
    epsb = sb(R_CONST + 2816, [128, 1], F32)
    cact = sb(R_CONST + 2848, [128, 16], BF16)
    c32 = sb(R_CONST + 2880, [128, 16], F32)
    modv = sb(R_CONST + 2944, [128, 2, 96], F32)
    adab = sb(R_CONST + 3712, [128, 2, 96], F32)
    g1 = sb(R_CONST + 4480, [128, 2, 16], F32)
    g2 = sb(R_CONST + 4608, [128, 2, 16], F32)
    gmod1 = sb(R_CONST + 4736, [128, 2, 16], F32)
    gmod2 = sb(R_CONST + 4864, [128, 2, 16], F32)
    fg = sb(R_CONST + 4992, [128, 16], F32)
    ngateb = sb(R_CONST + 5056, [128, 2, 4], F32)
    gw2_bf = sb(R_CONST + 5120, [16, 2, 512], BF16)
    gnormb = sb(R_CONST + 7168, [128, 2, 256], F32)
    CN = lambda k: nm("C", k)

    hT = sb(R_A, [128, KC, S], BF16, "hT")
    OG = sb(R_OG, [128, 8, S], BF16, "OG")
    OM = sb(R_OM, [128, 8, S], BF16, "OM")
    FF = sb(R_OG, [128, 15, S], BF16, "FF")
    WS = [sb(R_W + 4096 * s, [128, 2048], BF16, "W") for s in range(NSLOT)]

    def dump(name, ap, shape, dt, reads):
        if name not in dbg_names:
            return
        d = nc.dram_tensor("dbg_" + name, list(shape), dt, kind="ExternalOutput").ap()
        dbg_out[name] = d
        t = P.op("sp", lambda e, d=d, ap=ap: e.dma_start(out=d, in_=ap), reads=reads, writes=[nm("DR", "dbg", name)],
                 dma="S_dbg")
        final.append(t)

    final = []

    _sm = [0]

    def small_load(dst, src, wname):
        _sm[0] += 1
        P.op("sp", lambda e: e.dma_start(out=dst, in_=src, allow_slow_non_contiguous=True), writes=[wname],
             dma="S_sm%d" % _sm[0])

    def setup():
        gw2_32 = sb(R_T + 0, [16, 2, 512], F32)
        P.op("pool", lambda e: e.memset(ident32[:], 1.0), writes=[CN("ident32")])
        P.op("pool", lambda e: e.affine_select(out=ident32[:], in_=ident32[:], pattern=[[-1, 128]],
                                               compare_op=ALU.is_equal, fill=0.0, base=0, channel_multiplier=1),
             reads=[CN("ident32")], writes=[CN("ident32")])
        P.op("pool", lambda e: e.memset(maskU32[:], 1.0), writes=[CN("maskU32")])
        P.op("pool", lambda e: e.affine_select(out=maskU32[:], in_=maskU32[:], pattern=[[1, 128]],
                                               compare_op=ALU.is_ge, fill=0.0, base=0, channel_multiplier=-1),
             reads=[CN("maskU32")], writes=[CN("maskU32")])
        P.op("dve", lambda e: e.tensor_copy(out=ident_bf[:], in_=ident32[:]), reads=[CN("ident32")], writes=[CN("ident_bf")])
        P.op("dve", lambda e: e.tensor_copy(out=maskU_bf[:], in_=maskU32[:]), reads=[CN("maskU32")], writes=[CN("maskU_bf")])
        P.op("dve", lambda e: e.memset(ones32[:], 1.0), writes=[CN("ones32")])
        P.op("dve", lambda e: e.memset(ones_bf[:], 1.0), writes=[CN("ones_bf")])
        P.op("dve", lambda e: e.memset(epsb[:], EPS), writes=[CN("epsb")])

        P.op("dve", lambda e: e.memset(padmask[:], -1e30), writes=[CN("padmask")])
        for i in range(1, 8):
            P.op("dve", lambda e, i=i: e.memset(padmask[:, 2 * i:2 * i + 2, 0:i], 0.0), writes=[CN("padmask")])
        small_load(c32[:], c_d.rearrange("(kc p) -> p kc", p=128), CN("c32"))
        P.op("act", lambda e: e.activation(out=cact[:], in_=c32[:], func=AF.Silu), reads=[CN("c32")], writes=[CN("cact")])
        for l in range(DEPTH):
            small_load(adab[:, l, :], ada_b[l].rearrange("(j p) -> p j", p=128), CN("adab%d" % l))
            small_load(g1[:, l, :], norm1_g[l].rearrange("(j p) -> p j", p=128), CN("g1%d" % l))
            small_load(g2[:, l, :], norm2_g[l].rearrange("(j p) -> p j", p=128), CN("g2%d" % l))
            small_load(ngateb[:, l, :], gate_b[l].rearrange("(j p) -> p j", p=128), CN("ngateb%d" % l))
            small_load(gnormb[:, l, :], gnorm_g[l].partition_broadcast(128), CN("gnormb%d" % l))
            P.op("dve", lambda e, l=l: e.tensor_scalar(out=ngateb[:, l, :], in0=ngateb[:, l, :], scalar1=-1.0,
                                                       scalar2=None, op0=ALU.mult),
                 reads=[CN("ngateb%d" % l)], writes=[CN("ngateb%d" % l)])
        small_load(fg[:], final_g.rearrange("(j p) -> p j", p=128), CN("fg"))
        P.op("sp", lambda e: e.dma_start(out=gw2_32[:], in_=gate_w2.rearrange("l k c -> k l c")),
             writes=[nm("T", "gw2_32")], dma="S_smgw")
        P.op("dve", lambda e: e.tensor_copy(out=gw2_bf[:], in_=gw2_32[:]), reads=[nm("T", "gw2_32")], writes=[CN("gw2_bf")])

    jobs = []

    def wblock(w_ap, l, r0, nk, c0, ncols):
        return (w_ap[l, r0:r0 + nk * 128, c0:c0 + ncols].rearrange("(kc p) c -> p kc c", p=128), nk, ncols)

    bank_rot = [0]

    def next_bank(n=4, base=0):
        b = base + bank_rot[0] % n
        bank_rot[0] += 1
        return b

    evac_rot = [0]

    def evac_eng():
        evac_rot[0] += 1
        return "dve" if evac_rot[0] % 2 == 0 else "act"

    def copy_op(eng, out, in_, reads, writes, scale=None):
        if eng == "dve":
            if scale is None:
                P.op("dve", lambda e: e.tensor_copy(out=out, in_=in_), reads=reads, writes=writes)
            else:
                P.op("dve", lambda e: e.tensor_scalar(out=out, in0=in_, scalar1=scale, scalar2=None, op0=ALU.mult),
                     reads=reads, writes=writes)
        else:
            if scale is None:
                P.op("act", lambda e: e.copy(out=out, in_=in_), reads=reads, writes=writes)
            else:
                P.op("act", lambda e: e.activation(out=out, in_=in_, func=AF.Copy, scale=scale), reads=reads, writes=writes)

    HN = [nm("A", "h", cg) for cg in range(KC)]

    def mm_group(bank_ap, M, wv, nk, rhs_fn, reads, bank):
        def mm(e):
            r = None
            for kc in range(nk):
                r = e.matmul(bank_ap, lhsT=wv[:, kc, 0:M], rhs=rhs_fn(kc), start=(kc == 0), stop=(kc == nk - 1))
            return r
        P.op("pe", mm, reads=reads, writes=[BK(bank)])

    def ada_job(l, j):
        def fn(views, wn):
            wv = views[0]
            b = next_bank()
            mm_group(PS[b][:, 0:1], 128, wv, KC, lambda kc: cact[:, kc:kc + 1], [wn[0], CN("cact")], b)
            P.op("dve", lambda e: e.tensor_tensor(out=modv[:, l, j:j + 1], in0=PS[b][:, 0:1], in1=adab[:, l, j:j + 1], op=ALU.add),
                 reads=[BK(b), CN("adab%d" % l)], writes=[CN("mod%d_%d" % (l, j // 16 * 16))])
        return Job([wblock(ada_w, l, 0, KC, j * 128, 128)], fn)

    def gmod_job(l, which):
        def fn(views, wn):
            g, gm, o = (g1, gmod1, 16) if which == 1 else (g2, gmod2, 64)
            P.op("dve", lambda e: e.scalar_tensor_tensor(out=gm[:, l, :], in0=modv[:, l, o:o + 16], scalar=1.0,
                                                         in1=g[:, l, :], op0=ALU.add, op1=ALU.mult),
                 reads=[CN("mod%d_%d" % (l, o)), CN("g%d%d" % (which, l))], writes=[CN("gmod%d%d" % (which, l))])
        return Job([], fn)

    def x0_jobs():
        st = {}

        def mk(cg):
            def fn(views, wn):
                if cg == 0:
                    P.fence("T")
                    st["XS"] = [sb(R_T + 8192 * i, [128, 16, 128], F32) for i in range(3)]
                    st["SL"] = [sb(R_T + 24576 + 8192 * i, [128, S], F32) for i in range(2)]
                    st["SQ"] = [sb(R_T + 40960, [128, S], BF16)]
                xs, sl = st["XS"][cg % 3], st["SL"][cg % 2]
                xn = nm("T", "xs", cg % 3)
                P.op("sp", lambda e: e.dma_start(
                    out=xs[:], in_=x_d[:, cg * 128:(cg + 1) * 128].rearrange("(tt p) f -> p tt f", p=128)),
                    writes=[xn], dma="S_xs%d" % (cg % 3))
                for q in range(4):
                    def tr(e, q=q):
                        r = None
                        for k in range(4):
                            r = e.transpose(out=PS[q][:, k * 128:(k + 1) * 128], in_=xs[:, q * 4 + k, :], identity=ident32[:])
                        return r
                    P.op("pe", tr, reads=[xn, CN("ident32")], writes=[BK(q)])
                    copy_op(evac_eng(), sl[:, q * 512:(q + 1) * 512], PS[q][:], [BK(q)], [nm("T", "sl", cg % 2, q)])
                P.op("sp", lambda e: e.dma_start(out=R_d[cg], in_=sl[:]),
                     reads=[nm("T", "sl", cg % 2, q) for q in range(4)],
                     writes=[nm("DR", "R", cg, tc) for tc in range(NTC)], dma="S_sl%d" % (cg % 2))
                sq = st["SQ"][0]
                for q in range(4):
                    P.op("act", lambda e, q=q: e.activation(out=sq[:, q * 512:(q + 1) * 512], in_=sl[:, q * 512:(q + 1) * 512], func=AF.Square),
                         reads=[nm("T", "sl", cg % 2, q)], writes=[nm("T", "xsq", 0, q)])
                    P.op("pe", lambda e, q=q: e.matmul(PS[4 + q][:], lhsT=ones_bf[:], rhs=sq[:, q * 512:(q + 1) * 512],
                                                       start=(cg == 0), stop=(cg == KC - 1)),
                         reads=[nm("T", "xsq", 0, q), CN("ones_bf")], writes=[BK(4 + q)])
            return Job([], fn)
        return [mk(cg) for cg in range(KC)]

    def norm_jobs(l, which):
        def fn(views, wn):
            P.fence("T")
            NSL = 4
            SL = [sb(R_T + 8192 * i, [128, S], F32) for i in range(NSL)]
            SQ = [sb(R_T + 16384 + 4096 * i, [128, S], BF16) for i in range(2)]
            RS = sb(R_T + 32768, [128, S], F32)
            if which == 3:
                P.fence("OG")
                YS = [sb(R_OG + 8192 * i, [128, 16, 128], F32) for i in range(4)]
            k = [0]

            def load_slab(cg):
                i = k[0] % NSL
                k[0] += 1
                P.op("sp", lambda e: e.dma_start(out=SL[i][:], in_=R_d[cg]),
                     reads=[nm("DR", "R", cg, tc) for tc in range(NTC)], writes=[nm("T", "nsl", i)], dma="S_nsl%d" % i)
                return i
            for cg in range(KC if not FUSED_STATS else 0):
                i = load_slab(cg)
                P.op("act", lambda e, i=i: e.activation(out=SQ[i][:], in_=SL[i][:], func=AF.Square),
                     reads=[nm("T", "nsl", i)], writes=[nm("T", "nsq", i)])
                for tc in range(NTC):
                    P.op("pe", lambda e, i=i, tc=tc, cg=cg: e.matmul(
                        PS[4 + tc][:], lhsT=ones_bf[:], rhs=SQ[i][:, tc * 512:(tc + 1) * 512],
                        start=(cg == 0), stop=(cg == KC - 1)),
                        reads=[nm("T", "nsq", i), CN("ones_bf")], writes=[BK(4 + tc)])
            for tc in range(NTC):
                P.op("act", lambda e, tc=tc: e.activation(out=RS[:, tc * 512:(tc + 1) * 512], in_=PS[4 + tc][:],
                                                          func=AF.Sqrt, scale=1.0 / D, bias=epsb[:, 0:1]),
                     reads=[BK(4 + tc), CN("epsb")], writes=[nm("T", "rs", tc)])
                P.op("dve", lambda e, tc=tc: e.reciprocal(out=RS[:, tc * 512:(tc + 1) * 512], in_=RS[:, tc * 512:(tc + 1) * 512]),
                     reads=[nm("T", "rs", tc)], writes=[nm("T", "rs", tc)])
            rsn = [nm("T", "rs", tc) for tc in range(NTC)]
            if which == 3:
                scale_ap = lambda cg: fg[:, cg:cg + 1]
                sc_names = [CN("fg")]
            else:
                gm = gmod1 if which == 1 else gmod2
                so = 0 if which == 1 else 48
                scale_ap = lambda cg: gm[:, l, cg:cg + 1]
                sc_names = [CN("gmod%d%d" % (which, l)), CN("mod%d_%d" % (l, so))]
                P.fence("A")
            for cg in range(KC):
                i = load_slab(cg)
                P.op("dve", lambda e, i=i, cg=cg: e.scalar_tensor_tensor(
                    out=SL[i][:], in0=SL[i][:], scalar=scale_ap(cg), in1=RS[:], op0=ALU.mult, op1=ALU.mult),
                    reads=[nm("T", "nsl", i)] + rsn + sc_names, writes=[nm("T", "nsl", i)])
                if which != 3:
                    P.op("act", lambda e, i=i, cg=cg: e.activation(out=hT[:, cg, :], in_=SL[i][:], func=AF.Identity,
                                                                   bias=modv[:, l, so + cg:so + cg + 1]),
                         reads=[nm("T", "nsl", i)] + sc_names, writes=[HN[cg]])
                else:
                    ys = YS[cg % 4]
                    for q in range(4):
                        def tr(e, i=i, q=q):
                            r = None
                            for kk in range(4):
                                tt = q * 4 + kk
                                r = e.transpose(out=PS[q][:, kk * 128:(kk + 1) * 128], in_=SL[i][:, tt * 128:(tt + 1) * 128],
                                                identity=ident32[:])
                            return r
                        P.op("pe", tr, reads=[nm("T", "nsl", i), CN("ident32")], writes=[BK(q)])
                        copy_op(evac_eng(), ys[:, q * 4:(q + 1) * 4, :], PS[q][:].rearrange("p (a b) -> p a b", b=128),
                                [BK(q)], [nm("OG", "ys", cg % 4, q)])
                    t = P.op("sp", lambda e, ys=ys, cg=cg: e.dma_start(
                        out=y_d[:, cg * 128:(cg + 1) * 128].rearrange("(tt p) f -> p tt f", p=128), in_=ys[:]),
                        reads=[nm("OG", "ys", cg % 4, q) for q in range(4)], writes=[nm("DR", "y", cg)],
                        dma="S_ys%d" % (cg % 4))
                    final.append(t)
        return [Job([], fn)]

    def fm_steps(wv, wname, nk, M, act_ap, act_names, epilogue, nb=4):
        steps = []
        for tc in range(NTC):
            def st(tc=tc):
                b = next_bank(nb, 0)
                mm_group(PS[b][0:M, :], M, wv, nk, lambda kc, tc=tc: act_ap[:, kc, tc * 512:(tc + 1) * 512],
                         [wname] + act_names, b)
                epilogue(b, tc)
            steps.append(st)
        return steps

    def fm_job(load, M, act_ap, act_names, epilogue, banks=(0, 4)):
        def fn(views, wn):
            for st in fm_steps(views[0], wn[0], load[1], M, act_ap, act_names, epilogue, banks[1]):
                st()
        return Job([load], fn)

    def tm_steps(wv, wname, dst_fn, dst_names_fn, nb=4):
        steps = []
        for g in range(4):
            def st(g=g):
                b = next_bank(nb, 0)

                def mm(e, g=g, b=b):
                    r = None
                    for k in range(4):
                        tt = g * 4 + k
                        for kc in range(KC):
                            r = e.matmul(PS[b][:, k * 128:(k + 1) * 128], lhsT=hT[:, kc, tt * 128:(tt + 1) * 128],
                                         rhs=wv[:, kc, :], start=(kc == 0), stop=(kc == KC - 1))
                    return r
                P.op("pe", mm, reads=[wname] + HN, writes=[BK(b)])
                copy_op(evac_eng(), dst_fn(g), PS[b][:].rearrange("p (a b) -> p a b", b=128), [BK(b)], dst_names_fn(g))
            steps.append(st)
        return steps

    def interleave(gen, steps, first_n, per, skip=0):
        k = 0
        for i, _ in enumerate(gen):
            n = 0 if i < skip else (first_n if i == skip else per)
            for _ in range(n):
                if k < len(steps):
                    steps[k]()
                    k += 1
        while k < len(steps):
            steps[k]()
            k += 1

    def gla_jobs(l):
        out = []
        glrT = sb(R_OM + 0, [128, S], BF16)
        qT = sb(R_OM + 4096, [128, S], BF16)
        kT = sb(R_OM + 8192, [128, S], BF16)
        vhs = [sb(R_OM + 12288, [128, 16, 256], BF16), sb(R_T + 0, [128, 16, 256], BF16)]
        grSs = [sb(R_OM + 20480, [128, 2, S], BF16), sb(R_T + 8192, [128, 2, S], BF16)]
        Lbs = [sb(R_OM + 12288, [128, S], F32), sb(R_T + 0, [128, S], F32)]
        Cbs = [sb(R_OM + 20480, [128, S], F32), sb(R_T + 8192, [128, S], F32)]
        VA = [nm("OM", "va", 0), nm("T", "va", 1)]
        VB = [nm("OM", "vb", 0), nm("T", "vb", 1)]
        qt = sb(R_OM + 28672, [128, S], BF16)
        Eb = sb(R_T + 16384, [128, S], F32)
        kt = sb(R_T + 24576, [128, S], BF16)
        elast = sb(R_T + 28672, [128, 16], F32)
        attn_bf = [sb(R_T + 28800 + 256 * i, [128, 128], BF16) for i in range(2)]
        ktok = [sb(R_T + 29312 + 256 * i, [128, 128], BF16) for i in range(2)]
        state = sb(R_T + 29824, [128, 256], F32)
        tmpst = sb(R_T + 30848, [128, 256], F32)
        state_bf = [sb(R_T + 31872 + 512 * i, [128, 256], BF16) for i in range(2)]
        sqj = sb(R_T + 32896, [128, 256], F32)
        onb = [sb(R_T + 33920 + 512 * i, [128, 256], BF16) for i in range(2)]
        ssb = sb(R_T + 34944, [128, 4], F32)
        tmpexp = sb(R_T + 35008, [128, 512], F32)
        TN = lambda *k: nm("T", *k)
        MN = lambda *k: nm("OM", *k)

        def first(views, wn):
            P.fence("T")
            P.fence("OM")
            P.fence("OG")
        out.append(Job([], first))

        def ep_glr(b, tc):
            copy_op(evac_eng(), glrT[0:16, tc * 512:(tc + 1) * 512], PS[b][0:16, :], [BK(b)], [MN("glr", tc)])
        out.append(fm_job(wblock(w_in, l, 0, KC, OFF_GLR, 16), 16, hT, HN, ep_glr))

        def head_loads(h):
            return [wblock(w_in, l, 0, KC, OFF_GQ + h * 128, 128), wblock(w_in, l, 0, KC, OFF_GK + h * 128, 128),
                    wblock(w_in, l, 0, KC, OFF_GV + h * 256, 128), wblock(w_in, l, 0, KC, OFF_GR + h * 256, 128),
                    wblock(w_in, l, 0, KC, OFF_GV + h * 256 + 128, 128), wblock(w_in, l, 0, KC, OFF_GR + h * 256 + 128, 128)]

        def head_steps(h, views, wn):
            p = h % 2
            vh, grS = vhs[p], grSs[p]

            def ep_q(b, tc):
                copy_op(evac_eng(), qT[:, tc * 512:(tc + 1) * 512], PS[b][:], [BK(b)], [MN("q", tc)])

            def ep_k(b, tc):
                copy_op(evac_eng(), kT[:, tc * 512:(tc + 1) * 512], PS[b][:], [BK(b)], [MN("k", tc)])
            steps = fm_steps(views[0], wn[0], KC, 128, hT, HN, ep_q, 3) + fm_steps(views[1], wn[1], KC, 128, hT, HN, ep_k, 3)
            for half in range(2):
                steps += tm_steps(views[2 + 2 * half], wn[2 + 2 * half],
                                  lambda g, half=half: vh[:, g * 4:(g + 1) * 4, half * 128:(half + 1) * 128],
                                  lambda g: [VA[p]], 3)

                def ep_gr(b, tc, half=half):
                    P.op("act", lambda e: e.activation(out=grS[:, half, tc * 512:(tc + 1) * 512], in_=PS[b][:], func=AF.Silu),
                         reads=[BK(b)], writes=[VB[p]])
                steps += fm_steps(views[3 + 2 * half], wn[3 + 2 * half], KC, 128, hT, HN, ep_gr, 3)
            return steps

        def job0(views, wn):
            for st in head_steps(0, views, wn):
                st()
        out.append(Job(head_loads(0), job0))

        for h in range(4):
            def mixer(h=h):
                p = h % 2
                pp = (h + 1) % 2
                vh, grS, Lb, Cb = vhs[p], grSs[p], Lbs[pp], Cbs[pp]
                for tc in range(NTC):
                    b = next_bank()
                    P.op("pe", lambda e, b=b, tc=tc: e.matmul(PS[b][:], lhsT=gw2_bf[0:16, l, h * 128:(h + 1) * 128],
                                                              rhs=glrT[0:16, tc * 512:(tc + 1) * 512], start=True, stop=True),
                         reads=[CN("gw2_bf"), MN("glr", tc)], writes=[BK(b)])
                    P.op("act", lambda e, b=b: e.activation(out=tmpexp[:], in_=PS[b][:], func=AF.Exp, scale=-1.0,
                                                            bias=ngateb[:, l, h:h + 1]),
                         reads=[BK(b), CN("ngateb%d" % l)], writes=[TN("tmpexp")])
                    P.op("act", lambda e, tc=tc: e.activation(out=Lb[:, tc * 512:(tc + 1) * 512], in_=tmpexp[:], func=AF.Ln, bias=1.0),
                         reads=[TN("tmpexp")], writes=[VA[pp]])
                for c in range(16):
                    P.op("dve", lambda e, c=c: e.tensor_tensor_scan(out=Cb[:, c * 128:(c + 1) * 128], data0=ones32[:],
                                                                    data1=Lb[:, c * 128:(c + 1) * 128], initial=0.0,
                                                                    op0=ALU.mult, op1=ALU.add),
                         reads=[VA[pp], CN("ones32")], writes=[VB[pp]])
                CNs = [VB[pp]]
                P.op("act", lambda e: e.activation(out=Eb[:], in_=Cb[:], func=AF.Exp, scale=-1.0 / 16), reads=CNs, writes=[TN("E")])
                P.op("dve", lambda e: e.scalar_tensor_tensor(out=qt[:], in0=Eb[:], scalar=128.0 ** -0.5, in1=qT[:],
                                                             op0=ALU.mult, op1=ALU.mult),
                     reads=[TN("E")] + [MN("q", tc) for tc in range(NTC)], writes=[MN("qt")])
                P.op("act", lambda e: e.activation(out=elast[:], in_=Cb[:].rearrange("p (c t) -> p c t", t=128)[:, :, 127],
                                                   func=AF.Exp, scale=-1.0 / 16), reads=CNs, writes=[TN("elast")])
                P.op("act", lambda e: e.activation(out=Eb[:], in_=Cb[:], func=AF.Exp, scale=1.0 / 16),
                     reads=CNs + [TN("E")], writes=[TN("E")])
                P.op("dve", lambda e: e.tensor_tensor(out=kt[:], in0=Eb[:], in1=kT[:], op=ALU.mult),
                     reads=[TN("E")] + [MN("k", tc) for tc in range(NTC)], writes=[TN("kt")])
                yield
                B5b = PS[5][:].bitcast(BF16)
                vnames = [VA[p]]

                def stage_a(c):
                    ch = slice(c * 128, (c + 1) * 128)
                    i2 = c % 2
                    P.op("pe", lambda e: e.matmul(PS[4][:, 0:128], lhsT=kt[:, ch], rhs=qt[:, ch], start=True, stop=True),
                         reads=[TN("kt"), MN("qt")], writes=[BK(4)])
                    P.op("dve", lambda e: e.tensor_tensor(out=attn_bf[i2][:], in0=PS[4][:, 0:128], in1=maskU32[:], op=ALU.mult),
                         reads=[BK(4), CN("maskU32")], writes=[TN("attn", i2)])
                    if c < 15:
                        P.op("pe", lambda e: e.transpose(out=B5b[:, 0:128], in_=kt[:, ch], identity=ident_bf[:]),
                             reads=[TN("kt"), CN("ident_bf")], writes=[BK(5)])
                        P.op("act", lambda e: e.copy(out=ktok[i2][:], in_=B5b[:, 0:128]), reads=[BK(5)], writes=[TN("ktok", i2)])

                def stage_b(c):
                    ch = slice(c * 128, (c + 1) * 128)
                    i2 = c % 2
                    ob = 6 if c % 2 == 0 else 3

                    def omm(e):
                        r = e.matmul(PS[ob][:, 0:256], lhsT=attn_bf[i2][:], rhs=vh[:, c, :], start=True, stop=(c == 0))
                        if c > 0:
                            r = e.matmul(PS[ob][:, 0:256], lhsT=qt[:, ch], rhs=state_bf[(c - 1) % 2][:], start=False, stop=True)
                        return r
                    P.op("pe", omm, reads=[TN("attn", i2), MN("qt")] + vnames + ([TN("sbf", (c - 1) % 2)] if c > 0 else []),
                         writes=[BK(ob)])
                    if c < 15:
                        P.op("pe", lambda e: e.matmul(PS[7][:, 0:256], lhsT=ktok[i2][:], rhs=vh[:, c, :], start=True, stop=True),
                             reads=[TN("ktok", i2)] + vnames, writes=[BK(7)])
                        if c == 0:
                            P.op("dve", lambda e: e.tensor_copy(out=tmpst[:], in_=PS[7][:, 0:256]), reads=[BK(7)], writes=[TN("tmpst")])
                        else:
                            P.op("dve", lambda e: e.tensor_tensor(out=tmpst[:], in0=state[:], in1=PS[7][:, 0:256], op=ALU.add),
                                 reads=[BK(7), TN("state")], writes=[TN("tmpst")])
                        P.op("act", lambda e: e.activation(out=state_bf[c % 2][:], in_=tmpst[:], func=AF.Copy, scale=elast[:, c:c + 1]),
                             reads=[TN("tmpst"), TN("elast")], writes=[TN("sbf", c % 2)])
                        P.op("dve", lambda e: e.tensor_scalar(out=state[:], in0=tmpst[:], scalar1=elast[:, c:c + 1], scalar2=None, op0=ALU.mult),
                             reads=[TN("tmpst"), TN("elast")], writes=[TN("state")])
                    P.op("act", lambda e: e.activation(out=sqj[:], in_=PS[ob][:, 0:256], func=AF.Square, accum_out=ssb[:, 0:1]),
                         reads=[BK(ob)], writes=[TN("sqj"), TN("ss0")])
                    P.op("act", lambda e: e.activation(out=ssb[:, 1:2], in_=ssb[:, 0:1], func=AF.Sqrt, scale=1.0 / 256, bias=epsb[:, 0:1]),
                         reads=[TN("ss0"), CN("epsb")], writes=[TN("ss1")])
                    P.op("dve", lambda e: e.reciprocal(out=ssb[:, 2:3], in_=ssb[:, 1:2]), reads=[TN("ss1")], writes=[TN("ss2")])
                    P.op("dve", lambda e: e.scalar_tensor_tensor(out=onb[i2][:], in0=PS[ob][:, 0:256], scalar=ssb[:, 2:3],
                                                                 in1=gnormb[:, l, :], op0=ALU.mult, op1=ALU.mult),
                         reads=[BK(ob), TN("ss2"), CN("gnormb%d" % l)], writes=[TN("on", i2)])

                def stage_c(c):
                    ch = slice(c * 128, (c + 1) * 128)
                    i2 = c % 2

                    def otr(e):
                        e.transpose(out=B5b[:, 256:384], in_=onb[i2][:, 0:128], identity=ident_bf[:])
                        return e.transpose(out=B5b[:, 384:512], in_=onb[i2][:, 128:256], identity=ident_bf[:])
                    P.op("pe", otr, reads=[TN("on", i2), CN("ident_bf")], writes=[BK(5)])
                    P.op("dve", lambda e: e.tensor_tensor(
                        out=OG[:, 2 * h:2 * h + 2, ch], in0=B5b[:, 256:512].rearrange("p (a b) -> p a b", b=128),
                        in1=grS[:, :, ch], op=ALU.mult),
                        reads=[BK(5), VB[p]], writes=[nm("OG", "og", h, c)])

                stage_a(0)
                for c in range(16):
                    if c + 1 < 16:
                        stage_a(c + 1)
                    stage_b(c)
                    if c >= 1:
                        stage_c(c - 1)
                    yield
                stage_c(15)

            def run(views, wn, h=h, mixer=mixer):
                steps = head_steps(h + 1, views, wn) if h < 3 else []
                interleave(mixer(), steps, 8, 1)
            out.append(Job(head_loads(h + 1) if h < 3 else [], run))
        return out

    def moba_jobs(l):
        out = []
        qTs = [sb(R_T + 12416 * p + 0, [128, S], BF16) for p in range(2)]
        kTs = [sb(R_T + 12416 * p + 4096, [128, S], BF16) for p in range(2)]
        v1s = [sb(R_T + 12416 * p + 8192, [128, 16, 132], BF16) for p in range(2)]
        O2 = 12416
        km32 = sb(R_T + O2 + 12416, [128, 8], F32)
        km_bf = sb(R_T + O2 + 12480, [128, 8], BF16)
        sc_all = sb(R_T + O2 + 12544, [128, 16, 8], F32)
        top8 = sb(R_T + O2 + 13056, [128, 8], F32)
        sel = sb(R_T + O2 + 13120, [128, 16, 8], F32)
        Pb = [sb(R_T + O2 + 13632 + 1024 * i, [128, 512], BF16) for i in range(4)]
        accb = [sb(R_T + O2 + 17728 + 544 * i, [128, 132], F32) for i in range(2)]
        rden = sb(R_T + O2 + 18816, [128, 2], F32)
        otok = sb(R_T + O2 + 18848, [128, 16, 128], BF16)
        TN = lambda *k: nm("T", *k)

        def first(views, wn):
            P.fence("T")
            P.fence("OM")
            for p in range(2):
                P.op("dve", lambda e, p=p: e.memset(v1s[p][:, :, 128:132], 1.0), writes=[TN("v1ones", p)])
        out.append(Job([], first))

        def head_loads(h):
            return [wblock(w_in, l, 0, KC, OFF_MQ + h * 128, 128), wblock(w_in, l, 0, KC, OFF_MK + h * 128, 128),
                    wblock(w_in, l, 0, KC, OFF_MV + h * 128, 128)]

        def head_steps(h, views, wn):
            p = h % 2
            qT, kT, v1 = qTs[p], kTs[p], v1s[p]

            def ep_q(b, tc):
                copy_op(evac_eng(), qT[:, tc * 512:(tc + 1) * 512], PS[b][:], [BK(b)], [TN("q", p, tc)], scale=128.0 ** -0.5)

            def ep_k(b, tc):
                copy_op(evac_eng(), kT[:, tc * 512:(tc + 1) * 512], PS[b][:], [BK(b)], [TN("k", p, tc)])
            return (fm_steps(views[0], wn[0], KC, 128, hT, HN, ep_q, 2) + fm_steps(views[1], wn[1], KC, 128, hT, HN, ep_k, 2)
                    + tm_steps(views[2], wn[2], lambda g: v1[:, g * 4:(g + 1) * 4, 0:128], lambda g: [TN("v", p, g)], 2))

        def job0(views, wn):
            for st in head_steps(0, views, wn):
                st()
        out.append(Job(head_loads(0), job0))

        for h in range(8):
            def mixer(h=h):
                p = h % 2
                qT, kT, v1 = qTs[p], kTs[p], v1s[p]
                QN = [TN("q", p, tc) for tc in range(NTC)]
                KN = [TN("k", p, tc) for tc in range(NTC)]
                P.op("dve", lambda e: e.tensor_reduce(out=km32[:], in_=kT[:].rearrange("p (b t) -> p b t", t=256), axis=AX.X, op=ALU.add),
                     reads=KN, writes=[TN("km32")])
                P.op("dve", lambda e: e.tensor_scalar(out=km_bf[:], in0=km32[:], scalar1=1.0 / 256, scalar2=None, op0=ALU.mult),
                     reads=[TN("km32")], writes=[TN("km")])

                def bs(e):
                    r = None
                    for tt in range(16):
                        r = e.matmul(PS[3][:, tt * 8:(tt + 1) * 8], lhsT=qT[:, tt * 128:(tt + 1) * 128], rhs=km_bf[:], start=True, stop=True)
                    return r
                P.op("pe", bs, reads=QN + [TN("km")], writes=[BK(3)])
                P.op("dve", lambda e: e.tensor_tensor(out=sc_all[:], in0=PS[3][:, 0:128].rearrange("p (a b) -> p a b", b=8),
                                                      in1=padmask[:], op=ALU.add),
                     reads=[BK(3), CN("padmask")], writes=[TN("sc")])
                for tt in range(8, 16):
                    P.op("dve", lambda e, tt=tt: e.max(out=top8[:], in_=sc_all[:, tt, :]), reads=[TN("sc")], writes=[TN("top8")])
                    P.op("dve", lambda e, tt=tt: e.tensor_scalar(out=sel[:, tt, :], in0=sc_all[:, tt, :], scalar1=top8[:, 2:3],
                                                                 scalar2=None, op0=ALU.is_ge),
                         reads=[TN("sc"), TN("top8")], writes=[TN("sel", tt)])
                prot = [0]
                srot = [0]
                yield
                obanks = [6, 7, 3]

                def oview(ot):
                    bk = obanks[ot // 3]
                    return PS[bk][:, (ot % 3) * 132:(ot % 3) * 132 + 129], bk
                items = []
                for tq in range(16):
                    i = tq // 2
                    past = list(range(0, 2 * i))
                    groups = [past[g:g + 4] for g in range(0, len(past), 4)]
                    diag = [2 * i] if tq % 2 == 0 else [2 * i, 2 * i + 1]
                    allg = groups + [diag]
                    for gi, g in enumerate(allg):
                        items.append(dict(tq=tq, i=i, gi=gi, g=g, is_diag=(gi == len(allg) - 1), sparse=(i >= 4),
                                          sb=[2, 4, 5][len(items) % 3], pi=len(items) % 4))

                def emit_score(it):
                    tq, g, sb_, pi = it["tq"], it["g"], it["sb"], it["pi"]
                    tqs = slice(tq * 128, (tq + 1) * 128)
                    qn = [TN("q", p, tq // 4)]

                    def smm(e):
                        r = None
                        for k, ts in enumerate(g):
                            r = e.matmul(PS[sb_][:, k * 128:(k + 1) * 128], lhsT=kT[:, ts * 128:(ts + 1) * 128], rhs=qT[:, tqs],
                                         start=True, stop=True)
                        return r
                    P.op("pe", smm, reads=qn + KN, writes=[BK(sb_)])
                    n = len(g)
                    P.op("act", lambda e: e.activation(out=Pb[pi][:, 0:n * 128], in_=PS[sb_][:, 0:n * 128], func=AF.Exp),
                         reads=[BK(sb_)], writes=[TN("P", pi)])
                    if it["is_diag"]:
                        kd = len(g) - 1
                        P.op("dve", lambda e: e.tensor_tensor(out=Pb[pi][:, kd * 128:(kd + 1) * 128],
                                                              in0=Pb[pi][:, kd * 128:(kd + 1) * 128],
                                                              in1=maskU_bf[:], op=ALU.mult),
                             reads=[TN("P", pi), CN("maskU_bf")], writes=[TN("P", pi)])

                def emit_pv(it):
                    tq, i, gi, g, pi, is_diag, sparse = it["tq"], it["i"], it["gi"], it["g"], it["pi"], it["is_diag"], it["sparse"]
                    vn = [TN("P", pi), TN("v1ones", p)]
                    if not sparse or is_diag:
                        def pv(e):
                            r = None
                            ov, _ = oview(0)
                            for k, ts in enumerate(g):
                                st_ = (k == 0) if sparse else (gi == 0 and k == 0)
                                r = e.matmul(ov, lhsT=Pb[pi][:, k * 128:(k + 1) * 128], rhs=v1[:, ts, 0:129],
                                             start=st_, stop=(is_diag and k == len(g) - 1))
                            return r
                        P.op("pe", pv, reads=vn + [TN("v", p, ts // 4) for ts in g], writes=[BK(6)])
                    else:
                        for jj in range(len(g) // 2):
                            j = g[2 * jj] // 2
                            ov, bk = oview(1 + j)

                            def pv(e, jj=jj, ov=ov):
                                e.matmul(ov, lhsT=Pb[pi][:, (2 * jj) * 128:(2 * jj + 1) * 128], rhs=v1[:, g[2 * jj], 0:129], start=True, stop=False)
                                return e.matmul(ov, lhsT=Pb[pi][:, (2 * jj + 1) * 128:(2 * jj + 2) * 128], rhs=v1[:, g[2 * jj + 1], 0:129],
                                                start=False, stop=True)
                            P.op("pe", pv, reads=vn + [TN("v", p, ts // 4) for ts in g[2 * jj:2 * jj + 2]], writes=[BK(bk)])

                def finalize(tq):
                    i = tq // 2
                    if i < 4:
                        ov, _ = oview(0)
                        P.op("dve", lambda e: e.reciprocal(out=rden[:, 0:1], in_=ov[:, 128:129]), reads=[BK(6)], writes=[TN("rden")])
                        P.op("dve", lambda e: e.tensor_scalar(out=otok[:, tq, :], in0=ov[:, 0:128], scalar1=rden[:, 0:1],
                                                              scalar2=None, op0=ALU.mult),
                             reads=[BK(6), TN("rden")], writes=[TN("otok", tq)])
                    else:
                        ai = tq % 2
                        ov, _ = oview(0)
                        P.op("dve", lambda e: e.tensor_copy(out=accb[ai][:, 0:129], in_=ov), reads=[BK(6)], writes=[TN("acc", ai)])
                        for j in range(i):
                            ovj, bk = oview(1 + j)
                            P.op("dve", lambda e, ovj=ovj, j=j: e.scalar_tensor_tensor(
                                out=accb[ai][:, 0:129], in0=ovj, scalar=sel[:, tq, j:j + 1], in1=accb[ai][:, 0:129],
                                op0=ALU.mult, op1=ALU.add),
                                reads=[BK(bk), TN("sel", tq), TN("acc", ai)], writes=[TN("acc", ai)])
                        P.op("dve", lambda e: e.reciprocal(out=rden[:, 0:1], in_=accb[ai][:, 128:129]), reads=[TN("acc", ai)], writes=[TN("rden")])
                        P.op("dve", lambda e: e.tensor_scalar(out=otok[:, tq, :], in0=accb[ai][:, 0:128], scalar1=rden[:, 0:1],
                                                              scalar2=None, op0=ALU.mult),
                             reads=[TN("acc", ai), TN("rden")], writes=[TN("otok", tq)])

                emit_score(items[0])
                emit_score(items[1])
                for n_, it in enumerate(items):
                    if n_ + 2 < len(items):
                        emit_score(items[n_ + 2])
                    emit_pv(it)
                    if it["is_diag"]:
                        finalize(it["tq"])
                        yield
                for g in range(4):
                    bk = 4 + g % 2
                    Bb = PS[bk][:].bitcast(BF16)

                    def tr(e, g=g, Bb=Bb):
                        r = None
                        for k in range(4):
                            r = e.transpose(out=Bb[:, k * 128:(k + 1) * 128], in_=otok[:, g * 4 + k, :], identity=ident_bf[:])
                        return r
                    P.op("pe", tr, reads=[TN("otok", g * 4 + k) for k in range(4)] + [CN("ident_bf")], writes=[BK(bk)])
                    copy_op(evac_eng(), OM[:, h, g * 512:(g + 1) * 512], Bb[:, 0:512], [BK(bk)], [nm("OM", "om", h, g)])

            def run(views, wn, h=h, mixer=mixer):
                steps = head_steps(h + 1, views, wn) if h < 7 else []
                interleave(mixer(), steps, 1, 1, skip=5)
            out.append(Job(head_loads(h + 1) if h < 7 else [], run))
        return out

    def p5_jobs(l):
        out = []
        TN = lambda *k: nm("T", *k)
        tmp = [[sb(R_T + 8192 * s + 2048 * k, [128, 512], F32) for k in range(4)] for s in range(2)]
        MTS = [sb(R_T + 16384 + 4096 * i, [128, S], BF16) for i in range(2)]
        OGN = [nm("OG", "og", h, c) for h in range(4) for c in range(16)]
        OMN = [nm("OM", "om", h, g) for h in range(8) for g in range(4)]

        def first(views, wn):
            P.fence("T")
        out.append(Job([], first))
        cnt = [0]
        for fc in range(KC):
            def fn(views, wn, fc=fc):
                wug, wum, wgg, wgm = views
                ms = MTS[fc % 2]
                for tc in range(NTC):
                    s = cnt[0] % 2
                    cnt[0] += 1
                    bs_ = [4 * s + k for k in range(4)]
                    tsl = slice(tc * 512, (tc + 1) * 512)
                    mm_group(PS[bs_[0]][:], 128, wug, 8, lambda kc, tsl=tsl: OG[:, kc, tsl], [wn[0]] + OGN, bs_[0])
                    mm_group(PS[bs_[1]][:], 128, wum, 8, lambda kc, tsl=tsl: OM[:, kc, tsl], [wn[1]] + OMN, bs_[1])
                    mm_group(PS[bs_[2]][:], 128, wgg, KC, lambda kc, tsl=tsl: hT[:, kc, tsl], [wn[2]] + HN, bs_[2])
                    mm_group(PS[bs_[3]][:], 128, wgm, KC, lambda kc, tsl=tsl: hT[:, kc, tsl], [wn[3]] + HN, bs_[3])
                    sg, sm, t1, t2 = tmp[s]
                    P.op("act", lambda e, sg=sg, b=bs_[2]: e.activation(out=sg[:], in_=PS[b][:], func=AF.Sigmoid), reads=[BK(bs_[2])], writes=[TN("sg", s)])
                    P.op("act", lambda e, sm=sm, b=bs_[3]: e.activation(out=sm[:], in_=PS[b][:], func=AF.Sigmoid), reads=[BK(bs_[3])], writes=[TN("sm", s)])
                    P.op("dve", lambda e, t1=t1, sg=sg, b=bs_[0]: e.tensor_tensor(out=t1[:], in0=PS[b][:], in1=sg[:], op=ALU.mult),
                         reads=[BK(bs_[0]), TN("sg", s)], writes=[TN("t1", s)])
                    P.op("dve", lambda e, t2=t2, sm=sm, b=bs_[1]: e.tensor_tensor(out=t2[:], in0=PS[b][:], in1=sm[:], op=ALU.mult),
                         reads=[BK(bs_[1]), TN("sm", s)], writes=[TN("t2", s)])
                    P.op("dve", lambda e, t1=t1, t2=t2, ms=ms, tsl=tsl: e.tensor_tensor(out=ms[:, tsl], in0=t1[:], in1=t2[:], op=ALU.add),
                         reads=[TN("t1", s), TN("t2", s)], writes=[TN("mts", fc % 2, tc)])
                P.op("sp", lambda e, ms=ms: e.dma_start(out=MT_d[fc], in_=ms[:]),
                     reads=[TN("mts", fc % 2, tc) for tc in range(NTC)], writes=[nm("DR", "MT", fc)], dma="S_mts%d" % (fc % 2))
            loads = [wblock(w_up_gla, l, 0, 8, fc * 128, 128), wblock(w_up_moba, l, 0, 8, fc * 128, 128),
                     wblock(w_in, l, 0, KC, OFF_GG + fc * 128, 128), wblock(w_in, l, 0, KC, OFF_GM + fc * 128, 128)]
            out.append(Job(loads, fn))
        return out

    class RMW:
        def __init__(self, off, n=8, sq_off=None):
            self.tiles = [sb(off + 2048 * i, [128, 512], F32) for i in range(n)]
            self.sq = [sb(sq_off + 1024 * i, [128, 512], BF16) for i in range(4)] if sq_off is not None else None
            self.n = n
            self.k_load = 0
            self.k_use = 0
            self.order = []
            self.pending = []

        def plan(self, items):
            self.order = list(items)

        def prefetch(self, upto):
            while self.k_load < min(upto, len(self.order)):
                cg, tc, _ = self.order[self.k_load]
                i = self.k_load % self.n
                P.op("sp", lambda e, i=i, cg=cg, tc=tc: e.dma_start(out=self.tiles[i][:], in_=R_d[cg, :, tc * 512:(tc + 1) * 512]),
                     reads=[nm("DR", "R", cg, tc)], writes=[nm("T", "rt", i)], dma="S_rt%d" % i)
                self.k_load += 1

        def apply(self, bank, gate_ap, gate_names):
            k = self.k_use
            self.k_use += 1
            cg, tc, st = self.order[k]
            i = k % self.n
            self.prefetch(k + 1)
            P.op("dve", lambda e, i=i: e.scalar_tensor_tensor(out=self.tiles[i][:], in0=PS[bank][:], scalar=gate_ap,
                                                              in1=self.tiles[i][:], op0=ALU.mult, op1=ALU.add),
                 reads=[BK(bank), nm("T", "rt", i)] + gate_names, writes=[nm("T", "rt", i)])
            P.op("sp", lambda e, i=i, cg=cg, tc=tc: e.dma_start(out=R_d[cg, :, tc * 512:(tc + 1) * 512], in_=self.tiles[i][:]),
                 reads=[nm("T", "rt", i)], writes=[nm("DR", "R", cg, tc)], dma="S_rs%d" % i)
            if st:
                j = k % 4
                P.op("act", lambda e, i=i, j=j: e.activation(out=self.sq[j][:], in_=self.tiles[i][:], func=AF.Square),
                     reads=[nm("T", "rt", i)], writes=[nm("T", "rsq", j)])
                self.pending.append((j, tc, cg))
            last = (k == len(self.order) - 1)
            while self.pending and (len(self.pending) > 2 or last):
                j, tc2, cg2 = self.pending.pop(0)
                P.op("pe", lambda e, j=j, tc2=tc2, cg2=cg2: e.matmul(PS[4 + tc2][:], lhsT=ones_bf[:], rhs=self.sq[j][:],
                                                                     start=(cg2 == 0), stop=(cg2 == KC - 1)),
                     reads=[nm("T", "rsq", j), CN("ones_bf")], writes=[BK(4 + tc2)])
            self.prefetch(k + self.n - 1)

    def p6_jobs(l):
        out = []
        MTN = [nm("A", "mt", fc) for fc in range(KC)]
        rmw = [None]

        def first(views, wn):
            P.fence("T")
            P.fence("A")
            tk = None
            for fc in range(KC):
                tk = P.op("sp", lambda e, fc=fc: e.dma_start(out=hT[:, fc, :], in_=MT_d[fc]), reads=[nm("DR", "MT", fc)],
                          writes=[MTN[fc]], dma="S_mtl")
            for fc in range(KC):
                P.lastw[MTN[fc]] = tk
            rmw[0] = RMW(R_T + 0, sq_off=R_T + 16384)
            rmw[0].plan([(cg, tc, True) for cg in range(KC) for tc in range(NTC)])
            rmw[0].prefetch(7)
        out.append(Job([], first))
        for cg in range(KC):
            def ep(b, tc, cg=cg):
                rmw[0].apply(b, modv[:, l, 32 + cg:32 + cg + 1], [CN("mod%d_%d" % (l, 32))])
            out.append(fm_job(wblock(w_out, l, 0, KC, cg * 128, 128), 128, hT, MTN, ep))
        return out

    def ffn_jobs(l, extra=None):
        out = []
        TN = lambda *k: nm("T", *k)
        bounds = [round(q * NJ / NQ) for q in range(NQ + 1)]
        sa = [sb(R_T + 2048 * i, [128, 512], F32) for i in range(2)]
        rmw = [None]
        cnt = [0]

        def first(views, wn):
            P.fence("T")
            P.fence("OG")
            P.fence("OM")
            rmw[0] = RMW(R_T + 4096, sq_off=R_T + 20480)
            rmw[0].plan([(cg, tc, q == NQ - 1) for q in range(NQ) for cg in range(KC) for tc in range(NTC)])
        out.append(Job([], first))
        for q in range(NQ):
            j0, j1 = bounds[q], bounds[q + 1]
            nj = j1 - j0
            for j in range(j0, j1):
                def fn(views, wn, j=j, j0=j0):
                    wa, wu = views
                    jj = j - j0
                    for tc in range(NTC):
                        s = cnt[0] % 2
                        cnt[0] += 1
                        ba, bu = 2 * s, 2 * s + 1
                        tsl = slice(tc * 512, (tc + 1) * 512)
                        mm_group(PS[ba][:], 128, wa, KC, lambda kc, tsl=tsl: hT[:, kc, tsl], [wn[0]] + HN, ba)
                        mm_group(PS[bu][:], 128, wu, KC, lambda kc, tsl=tsl: hT[:, kc, tsl], [wn[1]] + HN, bu)
                        P.op("act", lambda e, s=s, ba=ba: e.activation(out=sa[s][:], in_=PS[ba][:], func=AF.Silu), reads=[BK(ba)], writes=[TN("sa", s)])
                        P.op("dve", lambda e, s=s, bu=bu, jj=jj, tsl=tsl: e.tensor_tensor(out=FF[:, jj, tsl], in0=PS[bu][:], in1=sa[s][:], op=ALU.mult),
                             reads=[BK(bu), TN("sa", s)], writes=[nm(("OG", "OM"), "ff", jj, tc)])
                out.append(Job([wblock(w_ffn_in, l, 0, KC, j * 128, 128), wblock(w_ffn_in, l, 0, KC, DFF + j * 128, 128)], fn))
                if extra:
                    out.append(extra.pop(0))
            for cg in range(KC):
                def fn2(views, wn, cg=cg, nj=nj, q=q):
                    wv = views[0]
                    if q == 0 and cg == 0:
                        rmw[0].prefetch(7)
                    for tc in range(NTC):
                        b = next_bank(4, 0) if q == NQ - 1 else 4 + next_bank(2, 0)
                        tsl = slice(tc * 512, (tc + 1) * 512)
                        mm_group(PS[b][:], 128, wv, nj, lambda kc, tsl=tsl: FF[:, kc, tsl],
                                 [wn[0]] + [nm(("OG", "OM"), "ff", jj, tc) for jj in range(nj)], b)
                        rmw[0].apply(b, modv[:, l, 80 + cg:80 + cg + 1], [CN("mod%d_%d" % (l, 80))])
                out.append(Job([wblock(w_ffn_out, l, j0 * 128, nj, cg * 128, 128)], fn2))
                if extra:
                    out.append(extra.pop(0))
            if q < NQ - 1:
                def fq(views, wn):
                    P.fence("OG")
                    P.fence("OM")
                out.append(Job([], fq))
        return out

    setup()

    def ada_list(l, j0, j1):
        return [ada_job(l, j) for j in range(j0, j1)]

    STAGES = ["mod", "x0", "n1", "gla", "moba", "p5", "p6", "n2", "ffn", "final"]

    def stage_idx(s):
        return STAGES.index(s)
    stop_i = stage_idx(stop) if stop else 10 ** 6
    nlayers = DEPTH
    def mix(main, extra, per):
        res = []
        extra = list(extra)
        for jb in main:
            res.append(jb)
            for _ in range(per):
                if extra:
                    res.append(extra.pop(0))
        return res + extra

    for l in range(nlayers):
        deferred = []
        if l == 0:
            early = ada_list(0, 0, 32) + [gmod_job(0, 1)]
            jobs += mix(x0_jobs(), early, 2)
            deferred = ada_list(0, 32, 96) + [gmod_job(0, 2)]
        jobs += norm_jobs(l, 1)
        if l == 0 and stop_i == stage_idx("n1"):
            break
        jobs += mix(gla_jobs(l), deferred[:14], 2)
        if l == 0 and stop_i == stage_idx("gla"):
            break
        jobs += mix(moba_jobs(l), deferred[14:34], 2)
        if l == 0 and stop_i == stage_idx("moba"):
            break
        jobs += mix(p5_jobs(l), deferred[34:], 2)
        if l == 0 and stop_i == stage_idx("p5"):
            break
        jobs += p6_jobs(l)
        if l == 0 and stop_i == stage_idx("p6"):
            break
        jobs += norm_jobs(l, 2)
        if l == 0 and stop_i == stage_idx("n2"):
            break
        extra = None
        if l == 0:
            extra = ada_list(1, 0, 96) + [gmod_job(1, 1), gmod_job(1, 2)]
        jobs += ffn_jobs(l, extra)
        if extra:
            jobs += extra
        if l == 0 and stop_i == stage_idx("ffn"):
            break
    else:
        jobs += norm_jobs(0, 3)

    load_q = []
    for ji, job in enumerate(jobs):
        for li in range(len(job.loads)):
            load_q.append((ji, li))
    issued = [0]

    def slot_view(k, nk, ncols):
        return WS[k % NSLOT][:, 0:nk * ncols].rearrange("p (k c) -> p k c", c=ncols)

    def issue_upto(n):
        while issued[0] < min(n, len(load_q)):
            k = issued[0]
            ji, li = load_q[k]
            ap, nk, ncols = jobs[ji].loads[li]
            dst = slot_view(k, nk, ncols)
            P.op("pool", lambda e, dst=dst, ap=ap: e.dma_start(out=dst, in_=ap), writes=[nm("W", k % NSLOT)],
                 dma="S_w%d" % (k % NSLOT))
            issued[0] += 1
    ptr = 0
    for ji, job in enumerate(jobs):
        nl = len(job.loads)
        issue_upto(ptr + NSLOT)
        views = [slot_view(ptr + li, job.loads[li][1], job.loads[li][2]) for li in range(nl)]
        wn = [nm("W", (ptr + li) % NSLOT) for li in range(nl)]
        job.fn(views, wn)
        ptr += nl

    dump("hT", hT[:], [128, KC, S], BF16, HN)
    dump("modv", modv[:], [128, 2, 96], F32, [CN("mod%d_%d" % (l, j)) for l in range(1) for j in range(0, 96, 16)])
    dump("OG", OG[:], [128, 8, S], BF16, [nm("OG", "og", h, c) for h in range(4) for c in range(16)])
    dump("OM", OM[:], [128, 8, S], BF16, [nm("OM", "om", h, g) for h in range(8) for g in range(4)])
    if "R" in dbg_names:
        d = nc.dram_tensor("dbg_R", [KC, 128, S], F32, kind="ExternalOutput").ap()
        for cg in range(KC):
            t = P.op("sp", lambda e, d=d, cg=cg: e.dma_start(out=d[cg], in_=R_d[cg]),
                     reads=[nm("DR", "R", cg, tc) for tc in range(NTC)], writes=[nm("DR", "dbgR", cg)], dma="S_dbg")
        final.append(t)
    if "MT" in dbg_names:
        d = nc.dram_tensor("dbg_MT", [KC, 128, S], BF16, kind="ExternalOutput").ap()
        for fc in range(KC):
            t = P.op("sp", lambda e, d=d, fc=fc: e.dma_start(out=d[fc], in_=MT_d[fc]), reads=[nm("DR", "MT", fc)],
                     writes=[nm("DR", "dbgMT", fc)], dma="S_dbg")
        final.append(t)
    if not final:
        pass
    P.emit(final)
    return nc


_NC_CACHE = {}


def kernel(x, c, ada_w, ada_b, norm1_g, w_in, gla_gate_w2, gla_gate_b, gla_norm_g, w_up_gla, w_up_moba, w_out,
           norm2_g, w_ffn_in, w_ffn_out, final_g):
    f = lambda a: np.ascontiguousarray(np.asarray(a, dtype=np.float32))
    x = f(x)
    c = f(c)
    shared = dict(ada_w=f(ada_w), ada_b=f(ada_b), norm1_g=f(norm1_g), w_in=f(w_in), gla_gate_w2=f(gla_gate_w2),
                  gla_gate_b=f(gla_gate_b), gla_norm_g=f(gla_norm_g), w_up_gla=f(w_up_gla), w_up_moba=f(w_up_moba),
                  w_out=f(w_out), norm2_g=f(norm2_g), w_ffn_in=f(w_ffn_in), w_ffn_out=f(w_ffn_out), final_g=f(final_g))
    nc = build_program()
    in_maps = []
    for b in range(8):
        m = dict(shared)
        m["x"] = x[b]
        m["c"] = c[b]
        in_maps.append(m)
    res = run_bass_kernel_spmd(nc, in_maps, core_ids=list(range(8)))
    return np.stack([np.asarray(r["y"], dtype=np.float32) for r in res.results], axis=0)
```
